# Optimizing a Trainium2 kernel written in Bass

```python
import jax
import jax.numpy as jnp
from jax import lax
import numpy as np

D_MODEL = 2048
BATCH = 4
SEQ = 4096
DEPTH = 2

HEAD_DIM = 128
ROPE_THETA = 10000.0
RMS_EPS = 1e-5
Q_BLOCK = 64

A_HEADS = 8
KV_RANK = 512
IDX_HEADS = 16
IDX_DIM = 64
TOPK_MAX = 256

B_PATTERNS = ((128, 1), (512, 4), (2048, 16))
B_GROUPS = 3
B_HEADS_PER_GROUP = 4

N_BRANCH = 2
N_MOD = 6

N_EXPERTS = 32
TOP_K = 4
D_FF = D_MODEL
SWIGLU_LIMIT = 7.0
SWIGLU_ALPHA = 1.702

A_Q_WIDTH = A_HEADS * HEAD_DIM
IDX_Q_WIDTH = IDX_HEADS * IDX_DIM
B_QKV_WIDTH = 3 * B_GROUPS * B_HEADS_PER_GROUP * HEAD_DIM
GATE_WIDTH = N_BRANCH * D_MODEL
IN_SIZES = (A_Q_WIDTH, KV_RANK, IDX_Q_WIDTH, IDX_DIM, IDX_HEADS, B_QKV_WIDTH, GATE_WIDTH)
D_IN = A_Q_WIDTH + KV_RANK + IDX_Q_WIDTH + IDX_DIM + IDX_HEADS + B_QKV_WIDTH + GATE_WIDTH
B_OUT_WIDTH = B_HEADS_PER_GROUP * HEAD_DIM

kernel_name = 'hybrid_dsa_dilated_moe_block'


def rmsnorm(x, g):
    xf = x.astype(jnp.float32)
    y = xf * lax.rsqrt(jnp.mean(xf * xf, axis=-1, keepdims=True) + RMS_EPS)
    return (y * g.astype(jnp.float32)).astype(x.dtype)


def modulate(h, shift, scale):
    return h * (1 + scale) + shift


def rope_tables(positions, dim):
    inv_freq = 1.0 / (ROPE_THETA ** (jnp.arange(0, dim, 2, dtype=jnp.float32) / dim))
    ang = positions.astype(jnp.float32)[..., None] * inv_freq
    return jnp.cos(ang), jnp.sin(ang)


def apply_rope(x, cos, sin):
    shape = cos.shape[:2] + (1,) * (x.ndim - 3) + cos.shape[-1:]
    cos = cos.reshape(shape)
    sin = sin.reshape(shape)
    x1, x2 = jnp.split(x.astype(jnp.float32), 2, axis=-1)
    return jnp.concatenate([x1 * cos - x2 * sin, x2 * cos + x1 * sin], axis=-1).astype(x.dtype)


def dsa_attention(q, k, v, q_idx, k_idx, w_idx):
    bsz, seq_len = q.shape[0], q.shape[1]
    n_sel = min(TOPK_MAX, seq_len // 4)
    idx_scale = (IDX_DIM ** -0.5) * (IDX_HEADS ** -0.5)
    attn_scale = HEAD_DIM ** -0.5
    key_pos = jnp.arange(seq_len)
    k_idx_f = k_idx.astype(jnp.float32)

    def block(i):
        t0 = i * Q_BLOCK
        t = t0 + jnp.arange(Q_BLOCK)
        qb = lax.dynamic_slice_in_dim(q, t0, Q_BLOCK, axis=1)
        qib = lax.dynamic_slice_in_dim(q_idx, t0, Q_BLOCK, axis=1).astype(jnp.float32)
        wib = lax.dynamic_slice_in_dim(w_idx, t0, Q_BLOCK, axis=1).astype(jnp.float32)
        dots = jnp.einsum('bqhd,bsd->bhqs', qib, k_idx_f)
        score = jnp.einsum('bhqs,bqh->bqs', jax.nn.relu(dots), wib) * idx_scale
        causal = key_pos[None, :] <= t[:, None]
        score = jnp.where(causal[None], score, -jnp.inf)
        _, sel = lax.top_k(score, n_sel)
        valid = sel <= t[None, :, None]
        kg = jax.vmap(lambda kk, ii: kk[ii])(k, sel)
        vg = jax.vmap(lambda vv, ii: vv[ii])(v, sel)
        logits = jnp.einsum('bqhd,bqkhd->bhqk', qb, kg, preferred_element_type=jnp.float32) * attn_scale
        logits = jnp.where(valid[:, None], logits, -jnp.inf)
        p = jax.nn.softmax(logits, axis=-1)
        o = jnp.einsum('bhqk,bqkhd->bqhd', p.astype(v.dtype), vg, preferred_element_type=jnp.float32)
        return o.astype(q.dtype)

    out = lax.map(block, jnp.arange(seq_len // Q_BLOCK))
    return jnp.moveaxis(out, 0, 1).reshape(bsz, seq_len, A_Q_WIDTH)


def dilated_attention(q, k, v):
    bsz, seq_len = q.shape[0], q.shape[1]
    scale = HEAD_DIM ** -0.5
    k_groups = [k[:, :, g] for g in range(B_GROUPS)]
    v_groups = [v[:, :, g] for g in range(B_GROUPS)]

    def block(i):
        t0 = i * Q_BLOCK
        t = t0 + jnp.arange(Q_BLOCK)
        qblk = lax.dynamic_slice_in_dim(q, t0, Q_BLOCK, axis=1)
        outs, lses = [], []
        for g, (window, dilation) in enumerate(B_PATTERNS):
            dist = dilation * jnp.arange(window // dilation + 1)
            pos = t[:, None] - dist[None, :]
            valid = pos >= 0
            pos = jnp.maximum(pos, 0)
            kg = jnp.take(k_groups[g], pos, axis=1)
            vg = jnp.take(v_groups[g], pos, axis=1)
            logits = jnp.einsum('bqhd,bqjhd->bhqj', qblk[:, :, g], kg, preferred_element_type=jnp.float32) * scale
            logits = jnp.where(valid[None, None], logits, -jnp.inf)
            lse = jax.nn.logsumexp(logits, axis=-1)
            p = jnp.exp(logits - lse[..., None])
            o = jnp.einsum('bhqj,bqjhd->bqhd', p.astype(v.dtype), vg, preferred_element_type=jnp.float32)
            outs.append(o)
            lses.append(lse)
        wg = jax.nn.softmax(jnp.stack(lses, axis=-1), axis=-1)
        wg = jnp.transpose(wg, (0, 2, 1, 3))[:, :, :, None, :]
        o = jnp.sum(jnp.stack(outs, axis=-1) * wg, axis=-1)
        return o.astype(q.dtype)

    out = lax.map(block, jnp.arange(seq_len // Q_BLOCK))
    return jnp.moveaxis(out, 0, 1).reshape(bsz, seq_len, B_OUT_WIDTH)


def hybrid_mixer(h, cos, sin, cos_i, sin_i, w_in, g_kv, w_kv_up, w_proj_a, w_proj_b, w_out):
    bsz, seq_len, _ = h.shape
    proj = h @ w_in
    offs = [int(o) for o in np.cumsum(IN_SIZES)[:-1]]
    qa, ckv, qi, ki, wi, qkv_b, gate_logits = jnp.split(proj, offs, axis=-1)
    qa = apply_rope(qa.reshape(bsz, seq_len, A_HEADS, HEAD_DIM), cos, sin)
    kv = (rmsnorm(ckv, g_kv) @ w_kv_up).reshape(bsz, seq_len, A_HEADS, 2, HEAD_DIM)
    ka = apply_rope(kv[:, :, :, 0], cos, sin)
    va = kv[:, :, :, 1]
    qi = apply_rope(qi.reshape(bsz, seq_len, IDX_HEADS, IDX_DIM), cos_i, sin_i)
    ki = apply_rope(ki, cos_i, sin_i)
    o_a = dsa_attention(qa, ka, va, qi, ki, wi)
    qkv_b = qkv_b.reshape(bsz, seq_len, 3, B_GROUPS, B_HEADS_PER_GROUP, HEAD_DIM)
    qb = apply_rope(qkv_b[:, :, 0], cos, sin)
    kb = apply_rope(qkv_b[:, :, 1], cos, sin)
    vb = qkv_b[:, :, 2]
    o_b = dilated_attention(qb, kb, vb)
    gates = jax.nn.sigmoid(gate_logits).reshape(bsz, seq_len, N_BRANCH, D_MODEL)
    merged = gates[:, :, 0] * (o_a @ w_proj_a) + gates[:, :, 1] * (o_b @ w_proj_b)
    return merged @ w_out


def moe_ffn(h, w_router, b_router, w1, b1, w2, b2):
    logits = (h @ w_router + b_router).astype(jnp.float32)
    top_val, top_idx = lax.top_k(logits, TOP_K)
    top_w = jax.nn.softmax(top_val, axis=-1)
    combine = jnp.sum(jax.nn.one_hot(top_idx, N_EXPERTS, dtype=jnp.float32) * top_w[..., None], axis=-2)
    out = jnp.zeros(h.shape, jnp.float32)
    for e in range(N_EXPERTS):
        a = h @ w1[e] + b1[e]
        glu = jnp.minimum(a[..., ::2], SWIGLU_LIMIT)
        lin = jnp.clip(a[..., 1::2], -SWIGLU_LIMIT, SWIGLU_LIMIT)
        act = glu * jax.nn.sigmoid(SWIGLU_ALPHA * glu) * (lin + 1)
        y = act @ w2[e] + b2[e]
        out = out + combine[..., e:e + 1] * y.astype(jnp.float32)
    return out.astype(h.dtype)


def setup_inputs(seed: int = 0) -> dict:
    key = jax.random.key(seed)
    ks = jax.random.split(key, 24)
    f32 = jnp.float32

    def nrm(k, shape, scale):
        return jax.random.normal(k, shape, f32) * scale

    x = jax.random.normal(ks[0], (BATCH, SEQ, D_MODEL), f32)
    c = jax.random.normal(ks[1], (BATCH, D_MODEL), f32)
    positions = jnp.arange(SEQ, dtype=jnp.int32)[None, :] + jax.random.randint(ks[2], (BATCH, 1), 0, 1024, dtype=jnp.int32)
    return {
        'x': x,
        'c': c,
        'positions': positions,
        'w_mod': nrm(ks[3], (DEPTH, D_MODEL, N_MOD * D_MODEL), 0.5 * D_MODEL ** -0.5),
        'b_mod': nrm(ks[4], (DEPTH, N_MOD * D_MODEL), 0.01),
        'g_norm1': 1.0 + nrm(ks[5], (DEPTH, D_MODEL), 0.02),
        'g_norm2': 1.0 + nrm(ks[6], (DEPTH, D_MODEL), 0.02),
        'w_in': nrm(ks[7], (DEPTH, D_MODEL, D_IN), D_MODEL ** -0.5),
        'g_kv': 1.0 + nrm(ks[8], (DEPTH, KV_RANK), 0.02),
        'w_kv_up': nrm(ks[9], (DEPTH, KV_RANK, A_HEADS * 2 * HEAD_DIM), KV_RANK ** -0.5),
        'w_proj_a': nrm(ks[10], (DEPTH, A_Q_WIDTH, D_MODEL), A_Q_WIDTH ** -0.5),
        'w_proj_b': nrm(ks[11], (DEPTH, B_OUT_WIDTH, D_MODEL), B_OUT_WIDTH ** -0.5),
        'w_out': nrm(ks[12], (DEPTH, D_MODEL, D_MODEL), D_MODEL ** -0.5),
        'w_router': nrm(ks[13], (DEPTH, D_MODEL, N_EXPERTS), D_MODEL ** -0.5),
        'b_router': nrm(ks[14], (DEPTH, N_EXPERTS), 0.01),
        'w_moe1': nrm(ks[15], (DEPTH, N_EXPERTS, D_MODEL, 2 * D_FF), D_MODEL ** -0.5),
        'b_moe1': nrm(ks[16], (DEPTH, N_EXPERTS, 2 * D_FF), 0.01),
        'w_moe2': nrm(ks[17], (DEPTH, N_EXPERTS, D_FF, D_MODEL), D_FF ** -0.5),
        'b_moe2': nrm(ks[18], (DEPTH, N_EXPERTS, D_MODEL), 0.01),
        'g_final': 1.0 + nrm(ks[19], (D_MODEL,), 0.02),
    }


def reference(x, c, positions, w_mod, b_mod, g_norm1, g_norm2, w_in, g_kv, w_kv_up, w_proj_a, w_proj_b, w_out, w_router, b_router, w_moe1, b_moe1, w_moe2, b_moe2, g_final):
    cos, sin = rope_tables(positions, HEAD_DIM)
    cos_i, sin_i = rope_tables(positions, IDX_DIM)
    c_act = jax.nn.silu(c)
    for l in range(DEPTH):
        mod = (c_act @ w_mod[l] + b_mod[l])[:, None, :]
        sh1, sc1, gt1, sh2, sc2, gt2 = jnp.split(mod, N_MOD, axis=-1)
        h = modulate(rmsnorm(x, g_norm1[l]), sh1, sc1)
        x = x + gt1 * hybrid_mixer(h, cos, sin, cos_i, sin_i, w_in[l], g_kv[l], w_kv_up[l], w_proj_a[l], w_proj_b[l], w_out[l])
        h = modulate(rmsnorm(x, g_norm2[l]), sh2, sc2)
        x = x + gt2 * moe_ffn(h, w_router[l], b_router[l], w_moe1[l], b_moe1[l], w_moe2[l], b_moe2[l])
    return rmsnorm(x, g_final)
```

```python
import numpy as np, time, os
RS = int(os.environ.get('KRS', '9')); CUT = int(os.environ.get('KCUT', '99')); NGRP = int(os.environ.get('KNG', '8'))
from contextlib import ExitStack
import ml_dtypes
import concourse.bass as bass
import concourse.mybir as mybir
from concourse.bass_utils import run_bass_kernel_spmd
F32 = mybir.dt.float32; BF16 = mybir.dt.bfloat16; I32 = mybir.dt.int32
AF = mybir.ActivationFunctionType; ALU = mybir.AluOpType; AX = mybir.AxisListType

D = 2048; S = 4096; DIN = 11344; NEG = -30000.0
PAT = ((128, 1), (512, 4), (2048, 16))


class Prog:
    NDS = 4
    def __init__(self, nc):
        self.nc = nc
        self.engs = {'pe': nc.tensor, 'act': nc.scalar, 'dve': nc.vector, 'pool': nc.gpsimd, 'sp': nc.sync}
        self.streams = {e: [] for e in self.engs}
        self.nops = {e: 0 for e in self.engs}
        self.ndma = {e: 0 for e in self.engs}
        self.lastw = {}; self.readers = {}
        self.waited = {e: {} for e in self.engs}
        self.alldma = {}
        self.sems = {}
        self.es = None
    def op(self, eng, fn, reads=(), writes=(), dma=False):
        writes = list(writes) + [k for k in reads if isinstance(k, tuple) and k[0] == 'ps' and k not in writes]
        deps = {}
        def add(tok):
            if tok is None: return
            k, v = tok
            if deps.get(k, 0) < v: deps[k] = v
        for k in reads: add(self.lastw.get(k))
        for k in writes:
            add(self.lastw.get(k))
            for sk, v in self.readers.get(k, {}).items(): add((sk, v))
        if dma:
            n = self.ndma[eng]; slot = n % self.NDS; rnd = n // self.NDS
            if rnd > 0: add((('d', eng, slot), 16 * rnd))
            tok = (('d', eng, slot), 16 * (rnd + 1)); self.ndma[eng] += 1
            self.alldma[tok[0]] = tok[1]
        else:
            self.nops[eng] += 1; tok = (('c', eng), self.nops[eng])
        waits = []
        for k, v in deps.items():
            if k == ('c', eng) and eng == 'pe': continue
            if self.waited[eng].get(k, 0) >= v: continue
            self.waited[eng][k] = v; waits.append((k, v))
        self.streams[eng].append((waits, fn, tok))
        for k in writes: self.lastw[k] = tok; self.readers[k] = {}
        for k in reads:
            r = self.readers.setdefault(k, {})
            if r.get(tok[0], 0) < tok[1]: r[tok[0]] = tok[1]
        return tok
    def sem(self, k):
        if k not in self.sems:
            self.sems[k] = self.es.enter_context(self.nc.semaphore("s_" + "_".join(str(x) for x in k)))
        return self.sems[k]
    def flush(self):
        nc = self.nc
        fin = dict(self.alldma)
        with nc.Block() as block:
            def mk(ename):
                eng = self.engs[ename]
                stream = self.streams[ename]
                def body(_e):
                    for waits, fn, tok in stream:
                        for k, v in waits: eng.wait_ge(self.sem(k), v)
                        inst = fn()
                        inst.then_inc(self.sem(tok[0]), 16 if tok[0][0] == 'd' else 1)
                    if ename == 'sp':
                        for k, v in fin.items(): eng.wait_ge(self.sem(k), v)
                return body
            block.tensor(mk('pe')); block.scalar(mk('act')); block.vector(mk('dve'))
            block.gpsimd(mk('pool')); block.sync(mk('sp'))
        self.streams = {e: [] for e in self.engs}


def build_program(NL=2, NE=32, phases="0ABCF", dbg=False):
    nc = bass.Bass("TRN2", target_bir_lowering=False)
    P = Prog(nc)
    def din(name, shape, dt=F32):
        return nc.dram_tensor(name, list(shape), dt, kind="ExternalInput").ap()
    def dscr(name, shape, dt=BF16):
        return nc.dram_tensor(name, list(shape), dt, kind="ExternalOutput" if dbg else "Internal").ap()
    x_in = din("x", [S, D]); cT_in = din("cT", [128, 16]); pos_in = din("pos", [1, S], I32)
    w_mod = din("w_mod", [NL, D, 6 * D]); bmodT = din("bmodT", [NL, 128, 96]); bmodgt = din("bmodgt", [NL, 2, 128, D])
    g1T = din("g1T", [NL, 128, 16]); g2T = din("g2T", [NL, 128, 16])
    w_in = din("w_in", [NL, D, DIN]); gkvT = din("gkvT", [NL, 128, 4]); w_kv = din("w_kv_up", [NL, 512, 2048])
    w_pa = din("w_proj_a", [NL, 1024, D]); w_pb = din("w_proj_b", [NL, 512, D]); w_o = din("w_out", [NL, D, D])
    w_r = din("w_router", [NL, D, 32]); brow = din("brow", [NL, 128, 32])
    w_m1 = din("w_moe1", [NL, NE, D, 2 * D]); b1T = din("b1T", [NL, NE, 128, 32])
    w_m2 = din("w_moe2", [NL, NE, D, D]); b2 = din("b_moe2", [NL, 32, D])
    gfin = din("gfin", [128, D])
    c_identf = din("identf", [128, 128]); c_identb = din("identb", [128, 128], BF16)
    c_sw128 = din("sw128", [128, 128], BF16); c_sw64 = din("sw64", [128, 128], BF16)
    c_tri = din("tri01", [128, 128]); c_atri = din("atri01", [128, 128])
    c_tab = din("dtab", [128, 42, 128], BF16); c_vec = din("cvec", [128, 8]); c_pow = din("pow2", [128, 24])
    out = nc.dram_tensor("out", [S, D], F32, kind="ExternalOutput").ap()
    QA = dscr("QA", [8, 128, S]); KA = dscr("KA", [8, 128, S]); VA = dscr("VA", [S, 1024])
    QI = dscr("QI", [8, 128, S]); KI = dscr("KI", [128, S]); WI = dscr("WI", [S, 16], F32)
    QB = dscr("QB", [12, 128, S]); KB = dscr("KB", [12, 128, S]); VB = dscr("VB", [S, 1536])
    GT = dscr("GT", [32, 128, S]); X1 = dscr("X1", [S, D], F32); XA = dscr("XA", [S, D], F32)

    es = ExitStack()
    P.es = es
    with es:
        def sb(name, shape, dt=F32):
            return es.enter_context(nc.sbuf_tensor("s_" + name, list(shape), dt))
        ps = [es.enter_context(nc.psum_tensor(f"ps{i}", [128, 512], F32)) for i in range(7)]
        psb16 = es.enter_context(nc.psum_tensor("psb16", [128, 1024], BF16))
        psk = [('ps', i) for i in range(8)]
        identf = sb("identf", [128, 128]); identb = sb("identb", [128, 128], BF16)
        sw128 = sb("sw128", [128, 128], BF16); sw64 = sb("sw64", [128, 128], BF16)
        tri01 = sb("tri01", [128, 128]); atri01 = sb("atri01", [128, 128])
        cvec = sb("cvec", [128, 8]); pow2 = sb("pow2", [128, 24])
        onesf = sb("onesf", [128, 128]); onesb = sb("onesb", [128, 128], BF16)
        modT = sb("modT", [128, 96]); G1 = sb("G1", [128, 16]); G2 = sb("G2", [128, 16])
        gtrow = sb("gtrow", [128, 2, D])
        for t, src in ((identf, c_identf), (identb, c_identb), (sw128, c_sw128), (sw64, c_sw64), (tri01, c_tri),
                       (atri01, c_atri), (cvec, c_vec), (pow2, c_pow)):
            P.op('sp', lambda t=t, src=src: nc.sync.dma_start(out=t[:], in_=src[:]), writes=[t.name], dma=True)
        P.op('dve', lambda: nc.vector.memset(onesf[:], 1.0), writes=['onesf'])
        P.op('dve', lambda: nc.vector.memset(onesb[:], 1.0), writes=['onesb'])
        P.flush()

        ctr = [0]
        def rot(n):
            ctr[0] += 1
            return ctr[0] % n

        def norm_group(es2, src, r0, ntile, Gv, SHv, hT, tagp):
            xt = es2['xt']; st = es2['st']
            for j in range(ntile):
                P.op('sp', lambda j=j: nc.sync.dma_start(out=xt[:, j, :], in_=src[r0 + j * 128:r0 + (j + 1) * 128, :]), writes=[('xt', j)], dma=True)
                P.op('act', lambda j=j: nc.scalar.activation(out=es2['junk'][:], in_=xt[:, j, :], func=AF.Square, accum_out=st[:, j:j + 1]), reads=[('xt', j)], writes=['junk', ('st', j)])
                P.op('act', lambda j=j: nc.scalar.activation(out=st[:, 8 + j:9 + j], in_=st[:, j:j + 1], func=AF.Sqrt, scale=1.0 / D, bias=cvec[:, 3:4]), reads=[('st', j)], writes=[('st2', j)])
                P.op('dve', lambda j=j: nc.vector.reciprocal(out=st[:, 16 + j:17 + j], in_=st[:, 8 + j:9 + j]), reads=[('st2', j)], writes=[('st3', j)])
                P.op('dve', lambda j=j: nc.vector.tensor_scalar(out=xt[:, j, :], in0=xt[:, j, :], scalar1=st[:, 16 + j:17 + j], scalar2=None, op0=ALU.mult), reads=[('st3', j), ('xt', j)], writes=[('xt', j)])
            for c in range(16):
                b = 5 + (c % 2)
                for j in range(ntile):
                    P.op('pe', lambda c=c, j=j, b=b: nc.tensor.transpose(out=ps[b][:, j * 128:(j + 1) * 128], in_=xt[:, j, c * 128:(c + 1) * 128], identity=identf[:]),
                         reads=[('xt', j), 'identf'], writes=[psk[b]])
                P.op('act', lambda c=c, b=b: nc.scalar.activation(out=hT[:, c, 0:128 * ntile], in_=ps[b][:, 0:128 * ntile], func=AF.Identity, scale=Gv[:, c:c + 1], bias=SHv[:, c:c + 1]),
                     reads=[psk[b], 'modT', 'G'], writes=[(tagp, c)])

        for l in range(NL):
            xsrc = x_in if l == 0 else XA
            xdst = XA if l == 0 else XA
            if '0' in phases:
                with ExitStack() as ph:
                    def sbp(name, shape, dt=F32):
                        return ph.enter_context(nc.sbuf_tensor(f"p{l}_" + name, list(shape), dt))
                    cact = sbp("cact", [128, 16]); cbc = sbp("cbc", [128, 16, 128]); c2 = sbp("c2", [128, 16, 2])
                    wm = sbp("wm", [128, 2, 16, 512]); bgt = sbp("bgt", [128, 2, D]); bmT = sbp("bmT", [128, 96]); gg = sbp("gg", [128, 32])
                    P.op('sp', lambda: nc.sync.dma_start(out=cact[:], in_=cT_in[:]), writes=['cact'], dma=True)
                    P.op('sp', lambda: nc.sync.dma_start(out=bgt[:], in_=bmodgt[l].rearrange("a p d -> p a d")), writes=['bgt'], dma=True)
                    P.op('sp', lambda: nc.sync.dma_start(out=bmT[:], in_=bmodT[l]), writes=['bmT'], dma=True)
                    P.op('sp', lambda: nc.sync.dma_start(out=gg[:, 0:16], in_=g1T[l]), writes=['gg1'], dma=True)
                    P.op('sp', lambda: nc.sync.dma_start(out=gg[:, 16:32], in_=g2T[l]), writes=['gg2'], dma=True)
                    P.op('act', lambda: nc.scalar.activation(out=cact[:], in_=cact[:], func=AF.Silu), reads=['cact'], writes=['cact'])
                    for k in range(16):
                        P.op('dve', lambda k=k: nc.vector.tensor_scalar(out=cbc[:, k, :], in0=onesf[:], scalar1=cact[:, k:k + 1], scalar2=None, op0=ALU.mult), reads=['cact', 'onesf'], writes=['cbc'])
                    for q in range(2):
                        P.op('dve', lambda q=q: nc.vector.tensor_copy(out=c2[:, :, q], in_=cact[:]), reads=['cact'], writes=['c2'])
                    col = 0
                    for n in range(24):
                        wb = n % 2
                        P.op('sp', lambda n=n, wb=wb: nc.sync.dma_start(out=wm[:, wb], in_=w_mod[l].rearrange("(c p) n -> p c n", p=128)[:, :, n * 512:(n + 1) * 512]), writes=[('wm', wb)], dma=True)
                        isgt = (8 <= n < 12) or (20 <= n < 24)
                        if isgt:
                            a = 0 if n < 12 else 1; jj = (n - 8) if n < 12 else (n - 20)
                            b = n % 2
                            for k in range(16):
                                P.op('pe', lambda k=k, wb=wb, b=b: nc.tensor.matmul(ps[b][:], lhsT=cbc[:, k, :], rhs=wm[:, wb, k, :], start=(k == 0), stop=(k == 15)), reads=['cbc', ('wm', wb)], writes=[psk[b]])
                            P.op('dve', lambda a=a, jj=jj, b=b: nc.vector.tensor_tensor(out=gtrow[:, a, jj * 512:(jj + 1) * 512], in0=ps[b][:], in1=bgt[:, a, jj * 512:(jj + 1) * 512], op=ALU.add), reads=[psk[b], 'bgt'], writes=['gtrow'])
                        else:
                            for sub in range(4):
                                ch = n * 4 + sub
                                for k in range(16):
                                    P.op('pe', lambda k=k, wb=wb, sub=sub, ch=ch: nc.tensor.matmul(ps[2][:, 2 * ch:2 * ch + 2], lhsT=wm[:, wb, k, sub * 128:(sub + 1) * 128], rhs=c2[:, k, :], start=(k == 0), stop=(k == 15)), reads=['c2', ('wm', wb)], writes=[psk[2]])
                    P.op('dve', lambda: nc.vector.memset(modT[:], 0.0), writes=['modT'])
                    for (c0, c1) in ((0, 32), (48, 80)):
                        P.op('dve', lambda c0=c0, c1=c1: nc.vector.tensor_tensor(out=modT[:, c0:c1], in0=ps[2][:, 2 * c0:2 * c1:2], in1=bmT[:, c0:c1], op=ALU.add), reads=[psk[2], 'bmT', 'modT'], writes=['modT'])
                    P.op('dve', lambda: nc.vector.scalar_tensor_tensor(out=G1[:], in0=modT[:, 16:32], scalar=1.0, in1=gg[:, 0:16], op0=ALU.add, op1=ALU.mult), reads=['modT', 'gg1'], writes=['G'])
                    P.op('dve', lambda: nc.vector.scalar_tensor_tensor(out=G2[:], in0=modT[:, 64:80], scalar=1.0, in1=gg[:, 16:32], op0=ALU.add, op1=ALU.mult), reads=['modT', 'gg2', 'G'], writes=['G'])
                    P.flush()
            if 'A' in phases:
                with ExitStack() as ph:
                    def sbp(name, shape, dt=F32):
                        return ph.enter_context(nc.sbuf_tensor(f"p{l}_" + name, list(shape), dt))
                    es2 = dict(xt=sbp("xt", [128, 4, D]), st=sbp("st", [128, 24]), junk=sbp("junk", [128, D], BF16))
                    hT = sbp("hT", [128, 16, 512], BF16)
                    wt = sbp("wt", [128, 2, 16, 512], BF16); wt80 = sbp("wt80", [128, 16, 80], BF16)
                    wkv = sbp("wkv", [128, 4, 2048], BF16); gkv = sbp("gkv", [128, 4])
                    ckr = sbp("ckr", [128, 4, 512]); cksq = sbp("cksq", [128, 4, 512], BF16); ckn = sbp("ckn", [128, 4, 512], BF16)
                    rbc = sbp("rbc", [128, 512])
                    posi = sbp("posi", [128, 512], I32); ang = sbp("ang", [128, 4, 512])
                    tabs = sbp("tabs", [128, 4, 512])
                    qbf = sbp("qbf", [128, 2, 512], BF16); t1 = sbp("t1", [128, 2, 512]); t2 = sbp("t2", [128, 2, 512])
                    osb = sbp("osb", [128, 4, 512], BF16); wisb = sbp("wisb", [128, 2, 16])
                    P.op('pool', lambda: nc.gpsimd.dma_start(out=wkv[:], in_=w_kv[l].rearrange("(c p) n -> p c n", p=128)), writes=['wkv'], dma=True)
                    P.op('sp', lambda: nc.sync.dma_start(out=gkv[:], in_=gkvT[l]), writes=['gkv'], dma=True)
                    P.op('pool', lambda: nc.gpsimd.dma_start(out=wt80[:], in_=w_in[l].rearrange("(c p) n -> p c n", p=128)[:, :, 2560:2640]), writes=['wt80'], dma=True)

                    def rope_store(psb, kind, dst_ap, extra_reads=()):
                        r = rot(2); o = rot(4)
                        if RS == 0: return
                        ci, si, swm = (0, 1, sw128) if kind == 128 else (2, 3, sw64)
                        P.op('act', lambda: nc.scalar.copy(out=qbf[:, r, :], in_=ps[psb][:]), reads=[psk[psb]], writes=[('qbf', r)])
                        if RS == 1: return
                        pb2 = 2 + r
                        P.op('pe', lambda: nc.tensor.matmul(ps[pb2][:], lhsT=swm[:], rhs=qbf[:, r, :], start=True, stop=True), reads=[('qbf', r), 'sw'], writes=[psk[pb2]])
                        if RS == 2: return
                        P.op('dve', lambda: nc.vector.tensor_tensor(out=t1[:, r, :], in0=ps[psb][:], in1=tabs[:, ci, :], op=ALU.mult), reads=[psk[psb], 'tabs'], writes=[('t1', r)])
                        P.op('dve', lambda: nc.vector.tensor_tensor(out=t2[:, r, :], in0=ps[pb2][:], in1=tabs[:, si, :], op=ALU.mult), reads=[psk[pb2], 'tabs'], writes=[('t2', r)])
                        if RS == 3: return
                        P.op('pool', lambda: nc.gpsimd.tensor_tensor(out=osb[:, o, :], in0=t1[:, r, :], in1=t2[:, r, :], op=ALU.add), reads=[('t1', r), ('t2', r)], writes=[('osb', o)])
                        P.op('sp', lambda: nc.sync.dma_start(out=dst_ap, in_=osb[:, o, :]), reads=[('osb', o)], writes=[dst_ap.tensor.name], dma=True)

                    for g in range(NGRP):
                        t0 = g * 512
                        norm_group(es2, xsrc, t0, 4, G1, modT[:, 0:16], hT, 'hT')
                        if CUT == 1: continue
                        P.op('sp', lambda t0=t0: nc.sync.dma_start(out=posi[:], in_=pos_in[0:1, t0:t0 + 512].partition_broadcast(128)), writes=['posi'], dma=True)
                        P.op('dve', lambda: nc.vector.tensor_copy(out=ang[:, 3, :], in_=posi[:]), reads=['posi'], writes=[('ang', 3)])
                        for kind_i, fcol in ((0, 0), (1, 1)):
                            sgn = cvec[:, 2:3]
                            def mkang(kind_i=kind_i, fcol=fcol):
                                a = ang[:, 0, :]; kf = ang[:, 1, :]; ki = posi
                                P.op('dve', lambda: nc.vector.tensor_scalar(out=a, in0=ang[:, 3, :], scalar1=cvec[:, fcol:fcol + 1], scalar2=None, op0=ALU.mult), reads=[('ang', 3), 'cvec'], writes=[('ang', 0)])
                                for which in (0, 1):
                                    if which == 1:
                                        P.op('dve', lambda: nc.vector.tensor_scalar(out=a, in0=a, scalar1=float(np.pi / 2), scalar2=None, op0=ALU.add), reads=[('ang', 0)], writes=[('ang', 0)])
                                    P.op('dve', lambda: nc.vector.tensor_scalar(out=kf, in0=a, scalar1=float(1 / (2 * np.pi)), scalar2=None, op0=ALU.mult), reads=[('ang', 0)], writes=[('ang', 1)])
                                    P.op('dve', lambda: nc.vector.tensor_copy(out=ki[:], in_=kf), reads=[('ang', 1)], writes=['posi'])
                                    P.op('dve', lambda: nc.vector.tensor_copy(out=kf, in_=ki[:]), reads=['posi'], writes=[('ang', 1)])
                                    r_ = ang[:, 2, :]
                                    P.op('dve', lambda: nc.vector.scalar_tensor_tensor(out=r_, in0=kf, scalar=-6.28125, in1=a, op0=ALU.mult, op1=ALU.add), reads=[('ang', 0), ('ang', 1)], writes=[('ang', 2)])
                                    P.op('dve', lambda: nc.vector.scalar_tensor_tensor(out=r_, in0=kf, scalar=-0.0019353071795864769, in1=r_, op0=ALU.mult, op1=ALU.add), reads=[('ang', 1), ('ang', 2)], writes=[('ang', 2)])
                                    P.op('dve', lambda: nc.vector.tensor_scalar(out=kf, in0=r_, scalar1=float(np.pi), scalar2=float(-2 * np.pi), op0=ALU.is_gt, op1=ALU.mult), reads=[('ang', 2)], writes=[('ang', 1)])
                                    P.op('dve', lambda: nc.vector.tensor_tensor(out=r_, in0=r_, in1=kf, op=ALU.add), reads=[('ang', 1), ('ang', 2)], writes=[('ang', 2)])
                                    P.op('dve', lambda: nc.vector.tensor_scalar(out=kf, in0=r_, scalar1=float(-np.pi), scalar2=float(2 * np.pi), op0=ALU.is_lt, op1=ALU.mult), reads=[('ang', 2)], writes=[('ang', 1)])
                                    P.op('dve', lambda: nc.vector.tensor_tensor(out=r_, in0=r_, in1=kf, op=ALU.add), reads=[('ang', 1), ('ang', 2)], writes=[('ang', 2)])
                                    P.op('dve', lambda: nc.vector.tensor_scalar(out=r_, in0=r_, scalar1=3.1415925, scalar2=-3.1415925, op0=ALU.min, op1=ALU.max), reads=[('ang', 2)], writes=[('ang', 2)])
                                    ti = 2 * kind_i + (1 - which)
                                    P.op('act', lambda ti=ti: nc.scalar.activation(out=tabs[:, ti, :], in_=r_, func=AF.Sin), reads=[('ang', 2)], writes=['tabs'])
                                    if which == 0:
                                        P.op('dve', lambda ti=ti: nc.vector.tensor_scalar(out=tabs[:, ti, :], in0=tabs[:, ti, :], scalar1=cvec[:, 2 + 2 * kind_i:3 + 2 * kind_i], scalar2=None, op0=ALU.mult), reads=['tabs', 'cvec'], writes=['tabs'])
                            mkang()
                        if CUT == 2: continue
                        def load_w(c0):
                            wb = rot(2)
                            P.op('pool', lambda: nc.gpsimd.dma_start(out=wt[:, wb], in_=w_in[l].rearrange("(c p) n -> p c n", p=128)[:, :, c0:c0 + 512]), writes=[('wt', wb)], dma=True)
                            return wb
                        def proj_fm(wb, sub, psb, wtile=None):
                            for k in range(16):
                                P.op('pe', lambda k=k: nc.tensor.matmul(ps[psb][:], lhsT=wt[:, wb, k, sub * 128:(sub + 1) * 128], rhs=hT[:, k, :], start=(k == 0), stop=(k == 15)),
                                     reads=[('wt', wb)] + [('hT', c) for c in range(16)], writes=[psk[psb]])
                        hreads = [('hT', c) for c in range(16)]
                        for cg in range(2):
                            wb = load_w(cg * 512)
                            for sub in range(4):
                                pb = rot(2); proj_fm(wb, sub, pb)
                                rope_store(pb, 128, QA[cg * 4 + sub][:, t0:t0 + 512])
                        if CUT == 3: continue
                        wb = load_w(1024)
                        for sub in range(4):
                            pb = rot(2); proj_fm(wb, sub, pb)
                            P.op('act', lambda sub=sub, pb=pb: nc.scalar.copy(out=ckr[:, sub, :], in_=ps[pb][:]), reads=[psk[pb]], writes=[('ckr', sub)])
                            P.op('act', lambda sub=sub: nc.scalar.activation(out=cksq[:, sub, :], in_=ckr[:, sub, :], func=AF.Square), reads=[('ckr', sub)], writes=[('cksq', sub)])
                        pb = rot(2)
                        for sub in range(4):
                            P.op('pe', lambda sub=sub, pb=pb: nc.tensor.matmul(ps[pb][:], lhsT=onesb[:], rhs=cksq[:, sub, :], start=(sub == 0), stop=(sub == 3)), reads=[('cksq', sub), 'onesb'], writes=[psk[pb]])
                        P.op('act', lambda pb=pb: nc.scalar.activation(out=rbc[:], in_=ps[pb][:], func=AF.Sqrt, scale=1.0 / 512, bias=cvec[:, 3:4]), reads=[psk[pb], 'cvec'], writes=['rbc'])
                        P.op('dve', lambda: nc.vector.reciprocal(out=rbc[:], in_=rbc[:]), reads=['rbc'], writes=['rbc'])
                        for sub in range(4):
                            P.op('dve', lambda sub=sub: nc.vector.scalar_tensor_tensor(out=ckn[:, sub, :], in0=ckr[:, sub, :], scalar=gkv[:, sub:sub + 1], in1=rbc[:], op0=ALU.mult, op1=ALU.mult), reads=[('ckr', sub), 'gkv', 'rbc'], writes=[('ckn', sub)])
                        cknr = [('ckn', s_) for s_ in range(4)]
                        for h in range(8):
                            pb = rot(2)
                            for k in range(4):
                                P.op('pe', lambda k=k, h=h, pb=pb: nc.tensor.matmul(ps[pb][:], lhsT=wkv[:, k, h * 256:h * 256 + 128], rhs=ckn[:, k, :], start=(k == 0), stop=(k == 3)), reads=cknr + ['wkv'], writes=[psk[pb]])
                            rope_store(pb, 128, KA[h][:, t0:t0 + 512])
                        wkv_v = [wkv[:, k, :].rearrange("p (h two d) -> p h two d", two=2, d=128) for k in range(4)]
                        for j in range(4):
                            for hh in range(2):
                                pb = rot(2); o = rot(4)
                                for k in range(4):
                                    P.op('pe', lambda k=k, j=j, hh=hh, pb=pb: nc.tensor.matmul(ps[pb][:].rearrange("p (h d) -> p h d", d=128), lhsT=ckn[:, k, j * 128:(j + 1) * 128], rhs=wkv_v[k][:, hh * 4:hh * 4 + 4, 1, :], start=(k == 0), stop=(k == 3)), reads=cknr + ['wkv'], writes=[psk[pb]])
                                P.op('act', lambda pb=pb, o=o: nc.scalar.copy(out=osb[:, o, :], in_=ps[pb][:]), reads=[psk[pb]], writes=[('osb', o)])
                                P.op('sp', lambda j=j, hh=hh, o=o, t0=t0: nc.sync.dma_start(out=VA[t0 + j * 128:t0 + (j + 1) * 128, hh * 512:(hh + 1) * 512], in_=osb[:, o, :]), reads=[('osb', o)], writes=['VA'], dma=True)
                        if CUT == 4: continue
                        for cg in range(2):
                            wb = load_w(1536 + cg * 512)
                            for sub in range(4):
                                pb = rot(2); proj_fm(wb, sub, pb)
                                rope_store(pb, 64, QI[cg * 4 + sub][:, t0:t0 + 512])
                        pb = rot(2)
                        for half in range(2):
                            for k in range(16):
                                P.op('pe', lambda k=k, half=half, pb=pb: nc.tensor.matmul(ps[pb][half * 64:(half + 1) * 64, :], lhsT=wt80[:, k, 0:64], rhs=hT[:, k, :], start=(k == 0), stop=(k == 15)), reads=hreads + ['wt80'], writes=[psk[pb]])
                        rope_store(pb, 64, KI[:, t0:t0 + 512])
                        for j in range(4):
                            pb = rot(2); o = rot(2)
                            for k in range(16):
                                P.op('pe', lambda k=k, j=j, pb=pb: nc.tensor.matmul(ps[pb][:, 0:16], lhsT=hT[:, k, j * 128:(j + 1) * 128], rhs=wt80[:, k, 64:80], start=(k == 0), stop=(k == 15)), reads=hreads + ['wt80'], writes=[psk[pb]])
                            P.op('act', lambda pb=pb, o=o: nc.scalar.mul(out=wisb[:, o, :], in_=ps[pb][:, 0:16], mul=float((64 ** -0.5) * (16 ** -0.5))), reads=[psk[pb]], writes=[('wisb', o)])
                            P.op('sp', lambda j=j, o=o, t0=t0: nc.sync.dma_start(out=WI[t0 + j * 128:t0 + (j + 1) * 128, :], in_=wisb[:, o, :]), reads=[('wisb', o)], writes=['WI'], dma=True)
                        if CUT == 5: continue
                        for which, dst in ((0, QB), (1, KB)):
                            for cg in range(3):
                                wb = load_w(2640 + which * 1536 + cg * 512)
                                for sub in range(4):
                                    pb = rot(2); proj_fm(wb, sub, pb)
                                    rope_store(pb, 128, dst[cg * 4 + sub][:, t0:t0 + 512])
                        for cg in range(3):
                            wb = load_w(5712 + cg * 512)
                            for j in range(4):
                                pb = rot(2); o = rot(4)
                                for k in range(16):
                                    P.op('pe', lambda k=k, j=j, pb=pb, wb=wb: nc.tensor.matmul(ps[pb][:], lhsT=hT[:, k, j * 128:(j + 1) * 128], rhs=wt[:, wb, k, :], start=(k == 0), stop=(k == 15)), reads=hreads + [('wt', wb)], writes=[psk[pb]])
                                P.op('act', lambda pb=pb, o=o: nc.scalar.copy(out=osb[:, o, :], in_=ps[pb][:]), reads=[psk[pb]], writes=[('osb', o)])
                                P.op('sp', lambda j=j, cg=cg, o=o, t0=t0: nc.sync.dma_start(out=VB[t0 + j * 128:t0 + (j + 1) * 128, cg * 512:(cg + 1) * 512], in_=osb[:, o, :]), reads=[('osb', o)], writes=['VB'], dma=True)
                        for cg in range(8):
                            wb = load_w(7248 + cg * 512)
                            for sub in range(4):
                                pb = rot(2); o = rot(4); proj_fm(wb, sub, pb)
                                P.op('act', lambda pb=pb, o=o: nc.scalar.activation(out=osb[:, o, :], in_=ps[pb][:], func=AF.Sigmoid), reads=[psk[pb]], writes=[('osb', o)])
                                P.op('sp', lambda cg=cg, sub=sub, o=o, t0=t0: nc.sync.dma_start(out=GT[cg * 4 + sub][:, t0:t0 + 512], in_=osb[:, o, :]), reads=[('osb', o)], writes=['GT'], dma=True)
                    P.flush()

            if 'B' in phases:
                with ExitStack() as phB:
                    def sbB(name, shape, dt=F32):
                        return phB.enter_context(nc.sbuf_tensor(f"b{l}_" + name, list(shape), dt))
                    dtab = sbB("dtab", [128, 42, 128], BF16)
                    mbT = sbB("mbT", [128, 32, 512], BF16)
                    oa = sbB("oa", [128, 8, 512], BF16); ob = sbB("ob", [128, 4, 512], BF16)
                    P.op('sp', lambda: nc.sync.dma_start(out=dtab[:], in_=c_tab[:]), writes=['dtab'], dma=True)
                    for G in range(NGRP):
                        q0 = G * 512; nkb = 4 * G + 4; nk = nkb * 128
                        with ExitStack() as ph:
                            def sbp(name, shape, dt=F32):
                                return ph.enter_context(nc.sbuf_tensor(f"ba{l}_{G}_" + name, list(shape), dt))
                            sc = sbp("sc", [128, S]); junkb = sbp("junkb", [128, S], BF16); mb01 = sbp("mb01", [128, 2, S], BF16)
                            qi_sb = sbp("qi", [128, 8, 512], BF16); ki_sb = sbp("ki", [128, S], BF16); wi_sb = sbp("wi", [128, 4, 16])
                            rl = sbp("rl", [128, 3, 512]); bs = sbp("bs", [128, 64]); dg = sbp("dg", [128, 128])
                            P.op('sp', lambda: nc.sync.dma_start(out=qi_sb[:], in_=QI[:, :, q0:q0 + 512].rearrange("c p t -> p c t")), reads=['QI'], writes=['qi_sb'], dma=True)
                            P.op('sp', lambda: nc.sync.dma_start(out=ki_sb[:, 0:nk], in_=KI[:, 0:nk]), reads=['KI'], writes=['ki_sb'], dma=True)
                            P.op('sp', lambda: nc.sync.dma_start(out=wi_sb[:], in_=WI[q0:q0 + 512, :].rearrange("(j p) h -> p j h", p=128)), reads=['WI'], writes=['wi_sb'], dma=True)
                            for j in range(4):
                                nkj = 128 * (4 * G + j + 1); jb = j % 2
                                nch = (nkj + 511) // 512
                                for ch in range(nch):
                                    kc = ch * 512; w = min(512, nkj - kc)
                                    for h in range(16):
                                        pb = rot(2); r3 = rot(3); hf = (h % 2) * 64
                                        P.op('pe', lambda h=h, pb=pb, hf=hf, kc=kc, w=w, j=j: nc.tensor.matmul(ps[pb][:, 0:w], lhsT=qi_sb[hf:hf + 64, h // 2, j * 128:(j + 1) * 128], rhs=ki_sb[hf:hf + 64, kc:kc + w], start=True, stop=True),
                                             reads=['qi_sb', 'ki_sb'], writes=[psk[pb]])
                                        P.op('act', lambda pb=pb, r3=r3, w=w: nc.scalar.activation(out=rl[:, r3, 0:w], in_=ps[pb][:, 0:w], func=AF.Relu), reads=[psk[pb]], writes=[('rl', r3)])
                                        if h == 0:
                                            P.op('dve', lambda r3=r3, kc=kc, w=w, j=j: nc.vector.tensor_scalar(out=sc[:, kc:kc + w], in0=rl[:, r3, 0:w], scalar1=wi_sb[:, j, 0:1], scalar2=None, op0=ALU.mult), reads=[('rl', r3), 'wi_sb'], writes=[('sc', ch)])
                                        else:
                                            P.op('dve', lambda r3=r3, kc=kc, w=w, j=j, h=h: nc.vector.scalar_tensor_tensor(out=sc[:, kc:kc + w], in0=rl[:, r3, 0:w], scalar=wi_sb[:, j, h:h + 1], in1=sc[:, kc:kc + w], op0=ALU.mult, op1=ALU.add), reads=[('rl', r3), 'wi_sb', ('sc', ch)], writes=[('sc', ch)])
                                scall = [('sc', ch) for ch in range(nch)]
                                V = lambda fn, rd, wr: P.op('dve', fn, reads=rd, writes=wr)
                                V(lambda nkj=nkj: nc.vector.tensor_reduce(out=bs[:, 0:1], in_=sc[:, 0:nkj], axis=AX.X, op=ALU.min), scall, ['bs'])
                                V(lambda nkj=nkj: nc.vector.tensor_reduce(out=bs[:, 1:2], in_=sc[:, 0:nkj], axis=AX.X, op=ALU.max), scall + ['bs'], ['bs'])
                                V(lambda: nc.vector.tensor_scalar(out=bs[:, 2:3], in0=bs[:, 0:1], scalar1=-1.0, scalar2=None, op0=ALU.add), ['bs'], ['bs'])
                                V(lambda: nc.vector.tensor_scalar(out=bs[:, 3:4], in0=bs[:, 0:1], scalar1=-0.5, scalar2=None, op0=ALU.add), ['bs'], ['bs'])
                                V(lambda: nc.vector.tensor_tensor(out=bs[:, 4:5], in0=bs[:, 1:2], in1=bs[:, 3:4], op=ALU.subtract), ['bs'], ['bs'])
                                V(lambda: nc.vector.tensor_scalar(out=bs[:, 8:32], in0=pow2[:], scalar1=bs[:, 4:5], scalar2=0.5, op0=ALU.mult, op1=ALU.mult), ['bs', 'pow2'], ['bs'])
                                V(lambda: nc.vector.tensor_tensor(out=bs[:, 5:6], in0=bs[:, 3:4], in1=bs[:, 8:9], op=ALU.add), ['bs'], ['bs'])
                                dk = nkj - 128; dch = dk // 512
                                V(lambda dk=dk: nc.vector.tensor_tensor(out=dg[:], in0=sc[:, dk:dk + 128], in1=tri01[:], op=ALU.mult), scall + ['tri01'], ['dg'])
                                V(lambda dk=dk: nc.vector.scalar_tensor_tensor(out=sc[:, dk:dk + 128], in0=atri01[:], scalar=bs[:, 2:3], in1=dg[:], op0=ALU.mult, op1=ALU.add), ['dg', 'bs', 'atri01'], [('sc', dch)])
                                for it in range(24):
                                    V(lambda nkj=nkj: nc.vector.tensor_scalar(out=junkb[:, 0:nkj], in0=sc[:, 0:nkj], scalar1=bs[:, 5:6], scalar2=None, op0=ALU.is_ge, op1=ALU.add, accum_out=bs[:, 6:7]), scall + ['bs'], ['junkb', 'bs'])
                                    V(lambda: nc.vector.tensor_scalar(out=bs[:, 7:8], in0=bs[:, 6:7], scalar1=255.5, scalar2=None, op0=ALU.is_ge), ['bs'], ['bs'])
                                    V(lambda it=it: nc.vector.scalar_tensor_tensor(out=bs[:, 3:4], in0=bs[:, 7:8], scalar=bs[:, 8 + it:9 + it], in1=bs[:, 3:4], op0=ALU.mult, op1=ALU.add), ['bs'], ['bs'])
                                    if it < 23:
                                        V(lambda it=it: nc.vector.tensor_tensor(out=bs[:, 5:6], in0=bs[:, 3:4], in1=bs[:, 9 + it:10 + it], op=ALU.add), ['bs'], ['bs'])
                                V(lambda nkj=nkj, jb=jb: nc.vector.tensor_scalar(out=mb01[:, jb, 0:nkj], in0=sc[:, 0:nkj], scalar1=bs[:, 3:4], scalar2=1.0, op0=ALU.is_ge, op1=ALU.subtract), scall + ['bs'], [('mb01', jb)])
                                if nkj < nk:
                                    P.op('pool', lambda nkj=nkj, jb=jb: nc.gpsimd.memset(mb01[:, jb, nkj:nk], -1.0), writes=[('mb01', jb)])
                                for kb0 in range(0, nkb, 4):
                                    nb = min(4, nkb - kb0); hb = rot(2) * 512
                                    for i in range(nb):
                                        P.op('pe', lambda i=i, kb0=kb0, jb=jb, hb=hb: nc.tensor.transpose(out=psb16[:, hb + i * 128:hb + (i + 1) * 128], in_=mb01[:, jb, (kb0 + i) * 128:(kb0 + i + 1) * 128], identity=identb[:]),
                                             reads=[('mb01', jb), 'identb'], writes=[psk[7]])
                                    P.op('act', lambda kb0=kb0, nb=nb, j=j, hb=hb: nc.scalar.mul(out=mbT[:, kb0:kb0 + nb, j * 128:(j + 1) * 128], in_=psb16[:, hb:hb + nb * 128].rearrange("p (n q) -> p n q", q=128), mul=-NEG),
                                         reads=[psk[7]], writes=[('mbT', kb0 + i) for i in range(nb)])
                            P.flush()
                        with ExitStack() as ph:
                            def sbp(name, shape, dt=F32):
                                return ph.enter_context(nc.sbuf_tensor(f"bb{l}_{G}_" + name, list(shape), dt))
                            kh = sbp("kh", [128, 2, S], BF16); vh = sbp("vh", [128, 2, 32, 128], BF16); qh = sbp("qh", [128, 2, 512], BF16)
                            pT = sbp("pT", [128, 3, 512], BF16); rinv = sbp("rinv", [128, 2, 512])
                            scale = float(128 ** -0.5)
                            def unit(kap, qap, mask_ap, vap, first, last, bo, bsum, rd, maskkeys):
                                pb = rot(2); r3 = rot(3)
                                P.op('pe', lambda: nc.tensor.matmul(ps[pb][:], lhsT=kap, rhs=qap, start=True, stop=False), reads=rd, writes=[psk[pb]])
                                P.op('pe', lambda: nc.tensor.matmul(ps[pb][:], lhsT=identb[:], rhs=mask_ap, start=False, stop=True), reads=maskkeys + ['identb'], writes=[psk[pb]])
                                P.op('act', lambda: nc.scalar.activation(out=pT[:, r3, :], in_=ps[pb][:], func=AF.Exp, scale=scale), reads=[psk[pb]], writes=[('pT', r3)])
                                P.op('pe', lambda: nc.tensor.matmul(ps[bo][:], lhsT=vap, rhs=pT[:, r3, :], start=first, stop=last), reads=rd + [('pT', r3)], writes=[psk[bo]])
                                P.op('pe', lambda: nc.tensor.matmul(ps[bsum][:], lhsT=onesb[:], rhs=pT[:, r3, :], start=first, stop=last), reads=[('pT', r3), 'onesb'], writes=[psk[bsum]])
                            def finalize(bo, bsum, dst, dkey):
                                rr_ = rot(2)
                                P.op('dve', lambda: nc.vector.reciprocal(out=rinv[:, rr_, :], in_=ps[bsum][:]), reads=[psk[bsum]], writes=[('rinv', rr_)])
                                P.op('dve', lambda: nc.vector.tensor_tensor(out=dst, in0=ps[bo][:], in1=rinv[:, rr_, :], op=ALU.mult), reads=[psk[bo], ('rinv', rr_)], writes=[dkey])
                            for h in range(8):
                                rr = h % 2
                                P.op('sp', lambda h=h, rr=rr: nc.sync.dma_start(out=qh[:, rr, :], in_=QA[h][:, q0:q0 + 512]), reads=['QA'], writes=[('qh', rr)], dma=True)
                                P.op('sp', lambda h=h, rr=rr: nc.sync.dma_start(out=kh[:, rr, 0:nk], in_=KA[h][:, 0:nk]), reads=['KA'], writes=[('kh', rr)], dma=True)
                                P.op('sp', lambda h=h, rr=rr: nc.sync.dma_start(out=vh[:, rr, 0:nkb, :], in_=VA[0:nk, h * 128:(h + 1) * 128].rearrange("(kb p) d -> p kb d", p=128)), reads=['VA'], writes=[('vh', rr)], dma=True)
                                for kb in range(nkb):
                                    unit(kh[:, rr, kb * 128:(kb + 1) * 128], qh[:, rr, :], mbT[:, kb, :], vh[:, rr, kb, :], kb == 0, kb == nkb - 1, 2 + rr, 4 + rr,
                                         [('qh', rr), ('kh', rr), ('vh', rr)], [('mbT', kb)])
                                finalize(2 + rr, 4 + rr, oa[:, h, :], ('oa', h))
                            B0 = 4 * G; toff = (0, 8, 19)
                            for hd in range(4):
                                units = []
                                for g, (W, dil) in enumerate(PAT):
                                    lo_kb = max(0, B0 - W // 128)
                                    for kb in range(lo_kb, B0 + 4):
                                        units.append((g, lo_kb, kb))
                                curg = -1
                                for ui, (g, lo_kb, kb) in enumerate(units):
                                    hh = g * 4 + hd
                                    if g != curg:
                                        curg = g; rr = rot(2)
                                        k0 = lo_kb * 128; k1 = (B0 + 4) * 128; nb_ = B0 + 4 - lo_kb
                                        P.op('sp', lambda hh=hh, rr=rr: nc.sync.dma_start(out=qh[:, rr, :], in_=QB[hh][:, q0:q0 + 512]), reads=['QB'], writes=[('qh', rr)], dma=True)
                                        P.op('sp', lambda hh=hh, rr=rr, k0=k0, k1=k1: nc.sync.dma_start(out=kh[:, rr, 0:k1 - k0], in_=KB[hh][:, k0:k1]), reads=['KB'], writes=[('kh', rr)], dma=True)
                                        P.op('sp', lambda hh=hh, rr=rr, k0=k0, k1=k1, nb_=nb_: nc.sync.dma_start(out=vh[:, rr, 0:nb_, :], in_=VB[k0:k1, hh * 128:(hh + 1) * 128].rearrange("(kb p) d -> p kb d", p=128)), reads=['VB'], writes=[('vh', rr)], dma=True)
                                    i = kb - lo_kb; d0 = B0 - kb; ti = toff[g] + d0 + 3
                                    unit(kh[:, rr, i * 128:(i + 1) * 128], qh[:, rr, :], dtab[:, ti:ti + 4, :].rearrange("p n q -> p (n q)"), vh[:, rr, i, :], ui == 0, ui == len(units) - 1, 2 + hd % 2, 4 + hd % 2,
                                         [('qh', rr), ('kh', rr), ('vh', rr)], ['dtab'])
                                finalize(2 + hd % 2, 4 + hd % 2, ob[:, hd, :], ('ob', hd))
                            P.flush()
                        with ExitStack() as ph:
                            def sbp(name, shape, dt=F32):
                                return ph.enter_context(nc.sbuf_tensor(f"bc{l}_{G}_" + name, list(shape), dt))
                            gsb = sbp("gsb", [128, 2, 2, 512], BF16); wpa = sbp("wpa", [128, 2, 8, 512], BF16); wpb = sbp("wpb", [128, 2, 4, 512], BF16)
                            mg = sbp("mg", [128, 16, 512], BF16); tm = sbp("tm", [128, 2, 2, 512]); wo = sbp("wo", [128, 2, 16, 512], BF16)
                            xr = sbp("xr", [128, 2, 512]); xo = sbp("xo", [128, 2, 512])
                            oar = [('oa', h) for h in range(8)]; obr = [('ob', h) for h in range(4)]
                            for cg in range(4):
                                w_ = cg % 2
                                P.op('pool', lambda cg=cg, w_=w_: nc.gpsimd.dma_start(out=wpa[:, w_], in_=w_pa[l].rearrange("(c p) n -> p c n", p=128)[:, :, cg * 512:(cg + 1) * 512]), writes=[('wpa', w_)], dma=True)
                                P.op('pool', lambda cg=cg, w_=w_: nc.gpsimd.dma_start(out=wpb[:, w_], in_=w_pb[l].rearrange("(c p) n -> p c n", p=128)[:, :, cg * 512:(cg + 1) * 512]), writes=[('wpb', w_)], dma=True)
                                for sub in range(4):
                                    c = cg * 4 + sub; r = c % 2; pa_ = rot(2); pb_ = 2 + rot(2)
                                    P.op('sp', lambda c=c, r=r: nc.sync.dma_start(out=gsb[:, r, 0, :], in_=GT[c][:, q0:q0 + 512]), reads=['GT'], writes=[('gsb', r, 0)], dma=True)
                                    P.op('sp', lambda c=c, r=r: nc.sync.dma_start(out=gsb[:, r, 1, :], in_=GT[16 + c][:, q0:q0 + 512]), reads=['GT'], writes=[('gsb', r, 1)], dma=True)
                                    for k in range(8):
                                        P.op('pe', lambda k=k, w_=w_, sub=sub, pa_=pa_: nc.tensor.matmul(ps[pa_][:], lhsT=wpa[:, w_, k, sub * 128:(sub + 1) * 128], rhs=oa[:, k, :], start=(k == 0), stop=(k == 7)), reads=oar + [('wpa', w_)], writes=[psk[pa_]])
                                    for k in range(4):
                                        P.op('pe', lambda k=k, w_=w_, sub=sub, pb_=pb_: nc.tensor.matmul(ps[pb_][:], lhsT=wpb[:, w_, k, sub * 128:(sub + 1) * 128], rhs=ob[:, k, :], start=(k == 0), stop=(k == 3)), reads=obr + [('wpb', w_)], writes=[psk[pb_]])
                                    P.op('dve', lambda r=r, pa_=pa_: nc.vector.tensor_tensor(out=tm[:, r, 0, :], in0=ps[pa_][:], in1=gsb[:, r, 0, :], op=ALU.mult), reads=[psk[pa_], ('gsb', r, 0)], writes=[('tm', r, 0)])
                                    P.op('dve', lambda r=r, pb_=pb_: nc.vector.tensor_tensor(out=tm[:, r, 1, :], in0=ps[pb_][:], in1=gsb[:, r, 1, :], op=ALU.mult), reads=[psk[pb_], ('gsb', r, 1)], writes=[('tm', r, 1)])
                                    P.op('pool', lambda r=r, c=c: nc.gpsimd.tensor_tensor(out=mg[:, c, :], in0=tm[:, r, 0, :], in1=tm[:, r, 1, :], op=ALU.add), reads=[('tm', r, 0), ('tm', r, 1)], writes=[('mg', c)])
                            mgr = [('mg', c) for c in range(16)]
                            for cg in range(4):
                                w_ = cg % 2
                                P.op('pool', lambda cg=cg, w_=w_: nc.gpsimd.dma_start(out=wo[:, w_], in_=w_o[l].rearrange("(c p) n -> p c n", p=128)[:, :, cg * 512:(cg + 1) * 512]), writes=[('wo', w_)], dma=True)
                                for j in range(4):
                                    pb = rot(2); r = rot(2)
                                    P.op('sp', lambda cg=cg, j=j, r=r: nc.sync.dma_start(out=xr[:, r, :], in_=xsrc[q0 + j * 128:q0 + (j + 1) * 128, cg * 512:(cg + 1) * 512]), writes=[('xr', r)], dma=True)
                                    for k in range(16):
                                        P.op('pe', lambda k=k, j=j, w_=w_, pb=pb: nc.tensor.matmul(ps[pb][:], lhsT=mg[:, k, j * 128:(j + 1) * 128], rhs=wo[:, w_, k, :], start=(k == 0), stop=(k == 15)), reads=mgr + [('wo', w_)], writes=[psk[pb]])
                                    P.op('dve', lambda cg=cg, pb=pb, r=r: nc.vector.tensor_tensor(out=xo[:, r, :], in0=ps[pb][:], in1=gtrow[:, 0, cg * 512:(cg + 1) * 512], op=ALU.mult), reads=[psk[pb], 'gtrow'], writes=[('xo', r)])
                                    P.op('pool', lambda r=r: nc.gpsimd.tensor_tensor(out=xo[:, r, :], in0=xo[:, r, :], in1=xr[:, r, :], op=ALU.add), reads=[('xo', r), ('xr', r)], writes=[('xo', r)])
                                    P.op('sp', lambda cg=cg, j=j, r=r: nc.sync.dma_start(out=X1[q0 + j * 128:q0 + (j + 1) * 128, cg * 512:(cg + 1) * 512], in_=xo[:, r, :]), reads=[('xo', r)], writes=['X1'], dma=True)
                            P.flush()

            if 'C' in phases:
                with ExitStack() as phC:
                    def sbC(name, shape, dt=F32):
                        return phC.enter_context(nc.sbuf_tensor(f"c{l}_" + name, list(shape), dt))
                    wr = sbC("wr", [128, 16, 32], BF16); browsb = sbC("brow", [128, 32]); b2sb = sbC("b2sb", [32, D])
                    h2T = sbC("h2T", [128, 16, 512], BF16); comb = sbC("comb", [128, 4, 32]); combT = sbC("combT", [32, 512])
                    acc = sbC("acc", [128, 4, D])
                    P.op('pool', lambda: nc.gpsimd.dma_start(out=wr[:], in_=w_r[l].rearrange("(c p) n -> p c n", p=128)), writes=['wr'], dma=True)
                    P.op('sp', lambda: nc.sync.dma_start(out=browsb[:], in_=brow[l]), writes=['browsb'], dma=True)
                    P.op('sp', lambda: nc.sync.dma_start(out=b2sb[:], in_=b2[l]), writes=['b2sb'], dma=True)
                    NPASS = NGRP
                    for p_ in range(NPASS):
                        t0 = p_ * 512
                        with ExitStack() as ph:
                            def sbp(name, shape, dt=F32):
                                return ph.enter_context(nc.sbuf_tensor(f"ca{l}_{p_}_" + name, list(shape), dt))
                            es2 = dict(xt=sbp("xt", [128, 4, D]), st=sbp("st", [128, 24]), junk=sbp("junk", [128, D], BF16))
                            rt = sbp("rt", [128, 8, 32])
                            norm_group(es2, X1, t0, 4, G2, modT[:, 48:64], h2T, 'h2T')
                            h2r = [('h2T', c) for c in range(16)]
                            for j in range(4):
                                pb = rot(2)
                                for k in range(16):
                                    P.op('pe', lambda k=k, j=j, pb=pb: nc.tensor.matmul(ps[pb][:, 0:32], lhsT=h2T[:, k, j * 128:(j + 1) * 128], rhs=wr[:, k, :], start=(k == 0), stop=(k == 15)), reads=h2r + ['wr'], writes=[psk[pb]])
                                V = lambda fn, rd, wr_: P.op('dve', fn, reads=rd, writes=wr_)
                                V(lambda pb=pb: nc.vector.tensor_tensor(out=rt[:, 0, :], in0=ps[pb][:, 0:32], in1=browsb[:], op=ALU.add), [psk[pb], 'browsb'], ['rt'])
                                V(lambda: nc.vector.max(out=rt[:, 1, 0:8], in_=rt[:, 0, :]), ['rt'], ['rt'])
                                V(lambda: nc.vector.tensor_scalar(out=rt[:, 1, 8:9], in0=rt[:, 1, 0:1], scalar1=-1.0, scalar2=None, op0=ALU.mult), ['rt'], ['rt'])
                                P.op('act', lambda: nc.scalar.activation(out=rt[:, 2, :], in_=rt[:, 0, :], func=AF.Exp, bias=rt[:, 1, 8:9]), reads=['rt'], writes=['rt'])
                                V(lambda: nc.vector.tensor_scalar(out=rt[:, 3, :], in0=rt[:, 0, :], scalar1=rt[:, 1, 3:4], scalar2=None, op0=ALU.is_ge), ['rt'], ['rt'])
                                V(lambda: nc.vector.tensor_tensor(out=rt[:, 2, :], in0=rt[:, 2, :], in1=rt[:, 3, :], op=ALU.mult), ['rt'], ['rt'])
                                V(lambda: nc.vector.tensor_reduce(out=rt[:, 1, 9:10], in_=rt[:, 2, :], axis=AX.X, op=ALU.add), ['rt'], ['rt'])
                                V(lambda: nc.vector.reciprocal(out=rt[:, 1, 10:11], in_=rt[:, 1, 9:10]), ['rt'], ['rt'])
                                V(lambda j=j: nc.vector.tensor_scalar(out=comb[:, j, :], in0=rt[:, 2, :], scalar1=rt[:, 1, 10:11], scalar2=None, op0=ALU.mult), ['rt'], [('comb', j)])
                                pb2 = 2 + rot(2)
                                P.op('pe', lambda j=j, pb2=pb2: nc.tensor.transpose(out=ps[pb2][0:32, 0:128], in_=comb[:, j, :], identity=identf[:]), reads=[('comb', j), 'identf'], writes=[psk[pb2]])
                                P.op('act', lambda j=j, pb2=pb2: nc.scalar.copy(out=combT[:, j * 128:(j + 1) * 128], in_=ps[pb2][0:32, 0:128]), reads=[psk[pb2]], writes=[('combT', j)])
                                for cg in range(4):
                                    pb3 = 4 + rot(2)
                                    P.op('pe', lambda j=j, cg=cg, pb3=pb3: nc.tensor.matmul(ps[pb3][:], lhsT=combT[:, j * 128:(j + 1) * 128], rhs=b2sb[:, cg * 512:(cg + 1) * 512], start=True, stop=True), reads=[('combT', j), 'b2sb'], writes=[psk[pb3]])
                                    P.op('act', lambda j=j, cg=cg, pb3=pb3: nc.scalar.copy(out=acc[:, j, cg * 512:(cg + 1) * 512], in_=ps[pb3][:]), reads=[psk[pb3]], writes=[('acc', j, cg)])
                            P.flush()
                        with ExitStack() as ph:
                            def sbp(name, shape, dt=F32):
                                return ph.enter_context(nc.sbuf_tensor(f"cb{l}_{p_}_" + name, list(shape), dt))
                            actT = sbp("actT", [128, 16, 512], BF16); w1t = sbp("w1t", [128, 2, 16, 512], BF16); w2t = sbp("w2t", [128, 2, 16, 512], BF16)
                            b1sb = sbp("b1sb", [128, 2, 32]); tg = sbp("tg", [128, 2, 4, 512])
                            h2r = [('h2T', c) for c in range(16)]
                            for e in range(NE):
                                er = e % 2
                                P.op('sp', lambda e=e, er=er: nc.sync.dma_start(out=b1sb[:, er, :], in_=b1T[l, e]), writes=[('b1sb', er)], dma=True)
                                for cc in range(8):
                                    w_ = rot(2)
                                    P.op('pool', lambda e=e, cc=cc, w_=w_: nc.gpsimd.dma_start(out=w1t[:, w_], in_=w_m1[l, e].rearrange("(k p) n -> p k n", p=128)[:, :, cc * 512:(cc + 1) * 512]), writes=[('w1t', w_)], dma=True)
                                    for sub in range(2):
                                        c = cc * 2 + sub; o0 = sub * 256
                                        pg = rot(2); pl = 2 + rot(2); r = rot(2)
                                        for k in range(16):
                                            P.op('pe', lambda k=k, w_=w_, pg=pg, o0=o0: nc.tensor.matmul(ps[pg][:], lhsT=w1t[:, w_, k, o0:o0 + 256:2], rhs=h2T[:, k, :], start=(k == 0), stop=(k == 15)), reads=h2r + [('w1t', w_)], writes=[psk[pg]])
                                        for k in range(16):
                                            P.op('pe', lambda k=k, w_=w_, pl=pl, o0=o0: nc.tensor.matmul(ps[pl][:], lhsT=w1t[:, w_, k, o0 + 1:o0 + 256:2], rhs=h2T[:, k, :], start=(k == 0), stop=(k == 15)), reads=h2r + [('w1t', w_)], writes=[psk[pl]])
                                        P.op('dve', lambda c=c, er=er, pg=pg, r=r: nc.vector.tensor_scalar(out=tg[:, r, 0, :], in0=ps[pg][:], scalar1=b1sb[:, er, c:c + 1], scalar2=7.0, op0=ALU.add, op1=ALU.min), reads=[psk[pg], ('b1sb', er)], writes=[('tg', r, 0)])
                                        P.op('act', lambda r=r: nc.scalar.activation(out=tg[:, r, 1, :], in_=tg[:, r, 0, :], func=AF.Sigmoid, scale=1.702), reads=[('tg', r, 0)], writes=[('tg', r, 1)])
                                        P.op('dve', lambda c=c, er=er, pl=pl, r=r: nc.vector.tensor_scalar(out=tg[:, r, 2, :], in0=ps[pl][:], scalar1=b1sb[:, er, 16 + c:17 + c], scalar2=7.0, op0=ALU.add, op1=ALU.min), reads=[psk[pl], ('b1sb', er)], writes=[('tg', r, 2)])
                                        P.op('dve', lambda r=r: nc.vector.tensor_scalar(out=tg[:, r, 2, :], in0=tg[:, r, 2, :], scalar1=-7.0, scalar2=1.0, op0=ALU.max, op1=ALU.add), reads=[('tg', r, 2)], writes=[('tg', r, 2)])
                                        P.op('pool', lambda r=r: nc.gpsimd.tensor_tensor(out=tg[:, r, 3, :], in0=tg[:, r, 0, :], in1=tg[:, r, 1, :], op=ALU.mult), reads=[('tg', r, 0), ('tg', r, 1)], writes=[('tg', r, 3)])
                                        P.op('pool', lambda r=r, c=c: nc.gpsimd.tensor_tensor(out=actT[:, c, :], in0=tg[:, r, 3, :], in1=tg[:, r, 2, :], op=ALU.mult), reads=[('tg', r, 3), ('tg', r, 2)], writes=[('actT', c)])
                                actr = [('actT', c) for c in range(16)]
                                for c2 in range(4):
                                    w_ = rot(2)
                                    P.op('pool', lambda e=e, c2=c2, w_=w_: nc.gpsimd.dma_start(out=w2t[:, w_], in_=w_m2[l, e].rearrange("(f p) n -> p f n", p=128)[:, :, c2 * 512:(c2 + 1) * 512]), writes=[('w2t', w_)], dma=True)
                                    for j in range(4):
                                        pb = 4 + rot(2)
                                        for f in range(16):
                                            P.op('pe', lambda f=f, j=j, w_=w_, pb=pb: nc.tensor.matmul(ps[pb][:], lhsT=actT[:, f, j * 128:(j + 1) * 128], rhs=w2t[:, w_, f, :], start=(f == 0), stop=(f == 15)), reads=actr + [('w2t', w_)], writes=[psk[pb]])
                                        P.op('dve', lambda j=j, c2=c2, e=e, pb=pb: nc.vector.scalar_tensor_tensor(out=acc[:, j, c2 * 512:(c2 + 1) * 512], in0=ps[pb][:], scalar=comb[:, j, e:e + 1], in1=acc[:, j, c2 * 512:(c2 + 1) * 512], op0=ALU.mult, op1=ALU.add),
                                             reads=[psk[pb], ('comb', j), ('acc', j, c2)], writes=[('acc', j, c2)])
                            P.flush()
                        with ExitStack() as ph:
                            def sbp(name, shape, dt=F32):
                                return ph.enter_context(nc.sbuf_tensor(f"cc{l}_{p_}_" + name, list(shape), dt))
                            xr = sbp("xr", [128, 2, D])
                            for j in range(4):
                                r = j % 2
                                P.op('sp', lambda j=j, r=r: nc.sync.dma_start(out=xr[:, r, :], in_=X1[t0 + j * 128:t0 + (j + 1) * 128, :]), reads=['X1'], writes=[('xr', r)], dma=True)
                                for cg in range(4):
                                    P.op('dve', lambda j=j, cg=cg: nc.vector.tensor_tensor(out=acc[:, j, cg * 512:(cg + 1) * 512], in0=acc[:, j, cg * 512:(cg + 1) * 512], in1=gtrow[:, 1, cg * 512:(cg + 1) * 512], op=ALU.mult), reads=[('acc', j, cg), 'gtrow'], writes=[('acc', j, cg)])
                                    P.op('pool', lambda j=j, cg=cg, r=r: nc.gpsimd.tensor_tensor(out=xr[:, r, cg * 512:(cg + 1) * 512], in0=xr[:, r, cg * 512:(cg + 1) * 512], in1=acc[:, j, cg * 512:(cg + 1) * 512], op=ALU.add), reads=[('acc', j, cg), ('xr', r)], writes=[('xr', r)])
                                P.op('sp', lambda j=j, r=r: nc.sync.dma_start(out=XA[t0 + j * 128:t0 + (j + 1) * 128, :], in_=xr[:, r, :]), reads=[('xr', r)], writes=['XA'], dma=True)
                            P.flush()
        if 'F' in phases:
            with ExitStack() as ph:
                xf = ph.enter_context(nc.sbuf_tensor("f_x", [128, 2, D], F32)); gf = ph.enter_context(nc.sbuf_tensor("f_g", [128, D], F32))
                sf = ph.enter_context(nc.sbuf_tensor("f_s", [128, 2, 4], F32)); jf = ph.enter_context(nc.sbuf_tensor("f_j", [128, D], BF16))
                P.op('sp', lambda: nc.sync.dma_start(out=gf[:], in_=gfin[:]), writes=['gf'], dma=True)
                for i in range(NGRP * 4):
                    r = i % 2
                    P.op('sp', lambda i=i, r=r: nc.sync.dma_start(out=xf[:, r, :], in_=XA[i * 128:(i + 1) * 128, :]), reads=['XA'], writes=[('xf', r)], dma=True)
                    P.op('act', lambda r=r: nc.scalar.activation(out=jf[:], in_=xf[:, r, :], func=AF.Square, accum_out=sf[:, r, 0:1]), reads=[('xf', r)], writes=['jf', ('sf', r)])
                    P.op('act', lambda r=r: nc.scalar.activation(out=sf[:, r, 1:2], in_=sf[:, r, 0:1], func=AF.Sqrt, scale=1.0 / D, bias=cvec[:, 3:4]), reads=[('sf', r)], writes=[('sf', r)])
                    P.op('dve', lambda r=r: nc.vector.reciprocal(out=sf[:, r, 2:3], in_=sf[:, r, 1:2]), reads=[('sf', r)], writes=[('sf', r)])
                    P.op('dve', lambda r=r: nc.vector.scalar_tensor_tensor(out=xf[:, r, :], in0=xf[:, r, :], scalar=sf[:, r, 2:3], in1=gf[:], op0=ALU.mult, op1=ALU.mult), reads=[('xf', r), ('sf', r), 'gf'], writes=[('xf', r)])
                    P.op('sp', lambda i=i, r=r: nc.sync.dma_start(out=out[i * 128:(i + 1) * 128, :], in_=xf[:, r, :]), reads=[('xf', r)], writes=['out'], dma=True)
                P.flush()
        P.flush()
    return nc


BF = ml_dtypes.bfloat16

def _consts():
    c = {}
    c["identf"] = np.eye(128, dtype=np.float32); c["identb"] = np.eye(128).astype(BF)
    sw = np.zeros((128, 128), np.float32)
    for m in range(128): sw[(m + 64) % 128, m] = 1
    c["sw128"] = sw.astype(BF)
    sw = np.zeros((128, 128), np.float32)
    for m in range(128): sw[(m // 64) * 64 + ((m % 64) + 32) % 64, m] = 1
    c["sw64"] = sw.astype(BF)
    t = np.arange(128)
    tri = (t[None, :] <= t[:, None]).astype(np.float32)
    c["tri01"] = tri; c["atri01"] = 1 - tri
    tabs = []
    for (W, d) in PAT:
        for delta in range(-3, W // 128 + 4):
            sk = np.arange(128)[:, None]; tq = np.arange(128)[None, :]
            diff = 128 * delta + tq - sk
            ok = (diff >= 0) & (diff <= W) & (diff % d == 0)
            tabs.append(np.where(ok, 0.0, NEG))
    c["dtab"] = np.stack(tabs, 1).astype(BF)
    cv = np.zeros((128, 8), np.float32); j = np.arange(128)
    cv[:, 0] = 1.0 / (10000.0 ** ((2 * (j % 64)).astype(np.float32) / 128))
    cv[:, 1] = 1.0 / (10000.0 ** ((2 * (j % 32)).astype(np.float32) / 64))
    cv[:, 2] = np.where(j < 64, -1.0, 1.0); cv[:, 3] = 1e-5; cv[:, 4] = np.where((j % 64) < 32, -1.0, 1.0)
    c["cvec"] = cv
    c["pow2"] = np.broadcast_to(0.5 ** np.arange(24, dtype=np.float32), (128, 24)).copy()
    return c


def kernel(x, c, positions, w_mod, b_mod, g_norm1, g_norm2, w_in, g_kv, w_kv_up, w_proj_a, w_proj_b, w_out,
           w_router, b_router, w_moe1, b_moe1, w_moe2, b_moe2, g_final):
    NL = 2; NE = 32
    f = lambda a: np.ascontiguousarray(np.asarray(a))
    x = f(x); c = f(c); positions = f(positions); b_mod = f(b_mod); b_moe1 = f(b_moe1)
    shared = dict(_consts())
    shared["w_mod"] = f(w_mod); shared["w_in"] = f(w_in); shared["w_kv_up"] = f(w_kv_up)
    shared["w_proj_a"] = f(w_proj_a); shared["w_proj_b"] = f(w_proj_b); shared["w_out"] = f(w_out)
    shared["w_router"] = f(w_router); shared["w_moe1"] = f(w_moe1); shared["w_moe2"] = f(w_moe2); shared["b_moe2"] = f(b_moe2)
    shared["brow"] = f(np.broadcast_to(np.asarray(b_router)[:, None, :], (NL, 128, NE)))
    b1g = b_moe1[:, :, 0::2].reshape(NL, NE, 16, 128).transpose(0, 1, 3, 2)
    b1l = b_moe1[:, :, 1::2].reshape(NL, NE, 16, 128).transpose(0, 1, 3, 2)
    shared["b1T"] = f(np.concatenate([b1g, b1l], -1))
    shared["gfin"] = f(np.broadcast_to(np.asarray(g_final)[None, :], (128, D)))
    shared["bmodT"] = f(b_mod.reshape(NL, 96, 128).transpose(0, 2, 1))
    gt = np.stack([b_mod[:, 2 * D:3 * D], b_mod[:, 5 * D:6 * D]], 1)
    shared["bmodgt"] = f(np.broadcast_to(gt[:, :, None, :], (NL, 2, 128, D)))
    shared["g1T"] = f(np.asarray(g_norm1).reshape(NL, 16, 128).transpose(0, 2, 1))
    shared["g2T"] = f(np.asarray(g_norm2).reshape(NL, 16, 128).transpose(0, 2, 1))
    shared["gkvT"] = f(np.asarray(g_kv).reshape(NL, 4, 128).transpose(0, 2, 1))
    in_maps = []
    for b in range(4):
        m = dict(shared)
        m["x"] = f(x[b]); m["cT"] = f(c[b].reshape(16, 128).T); m["pos"] = f(positions[b][None, :].astype(np.int32))
        in_maps.append(m)
    nc = build_program(NL=NL, NE=NE, phases="0ABCF")
    res = run_bass_kernel_spmd(nc, in_maps, core_ids=list(range(4)))
    return np.stack([np.asarray(r["out"]) for r in res.results], 0).astype(np.float32)
```

```python
import numpy as np, time, os
RS = int(os.environ.get('KRS', '9')); CUT = int(os.environ.get('KCUT', '99')); NGRP = int(os.environ.get('KNG', '8'))
from contextlib import ExitStack
import ml_dtypes
import concourse.bass as bass
import concourse.mybir as mybir
from concourse.bass_utils import run_bass_kernel_spmd
F32 = mybir.dt.float32; BF16 = mybir.dt.bfloat16; I32 = mybir.dt.int32
AF = mybir.ActivationFunctionType; ALU = mybir.AluOpType; AX = mybir.AxisListType

D = 2048; S = 4096; DIN = 11344; NEG = -30000.0
PAT = ((128, 1), (512, 4), (2048, 16))


class Prog:
    NDS = 4
    def __init__(self, nc):
        self.nc = nc
        self.engs = {'pe': nc.tensor, 'act': nc.scalar, 'dve': nc.vector, 'pool': nc.gpsimd, 'sp': nc.sync}
        self.streams = {e: [] for e in self.engs}
        self.nops = {e: 0 for e in self.engs}
        self.ndma = {e: 0 for e in self.engs}
        self.lastw = {}; self.readers = {}
        self.waited = {e: {} for e in self.engs}
        self.alldma = {}
        self.sems = {}
        self.es = None
    def op(self, eng, fn, reads=(), writes=(), dma=False):
        writes = list(writes) + [k for k in reads if isinstance(k, tuple) and k[0] == 'ps' and k not in writes]
        deps = {}
        def add(tok):
            if tok is None: return
            k, v = tok
            if deps.get(k, 0) < v: deps[k] = v
        for k in reads: add(self.lastw.get(k))
        for k in writes:
            add(self.lastw.get(k))
            for sk, v in self.readers.get(k, {}).items(): add((sk, v))
        if dma:
            n = self.ndma[eng]; slot = n % self.NDS; rnd = n // self.NDS
            if rnd > 0: add((('d', eng, slot), 16 * rnd))
            tok = (('d', eng, slot), 16 * (rnd + 1)); self.ndma[eng] += 1
            self.alldma[tok[0]] = tok[1]
        else:
            self.nops[eng] += 1; tok = (('c', eng), self.nops[eng])
        waits = []
        for k, v in deps.items():
            if k == ('c', eng) and eng == 'pe': continue
            if self.waited[eng].get(k, 0) >= v: continue
            self.waited[eng][k] = v; waits.append((k, v))
        self.streams[eng].append((waits, fn, tok))
        for k in writes: self.lastw[k] = tok; self.readers[k] = {}
        for k in reads:
            r = self.readers.setdefault(k, {})
            if r.get(tok[0], 0) < tok[1]: r[tok[0]] = tok[1]
        return tok
    def sem(self, k):
        if k not in self.sems:
            self.sems[k] = self.es.enter_context(self.nc.semaphore("s_" + "_".join(str(x) for x in k)))
        return self.sems[k]
    def flush(self):
        nc = self.nc
        fin = dict(self.alldma)
        with nc.Block() as block:
            def mk(ename):
                eng = self.engs[ename]
                stream = self.streams[ename]
                def body(_e):
                    for waits, fn, tok in stream:
                        for k, v in waits: eng.wait_ge(self.sem(k), v)
                        inst = fn()
                        inst.then_inc(self.sem(tok[0]), 16 if tok[0][0] == 'd' else 1)
                    if ename == 'sp':
                        for k, v in fin.items(): eng.wait_ge(self.sem(k), v)
                return body
            block.tensor(mk('pe')); block.scalar(mk('act')); block.vector(mk('dve'))
            block.gpsimd(mk('pool')); block.sync(mk('sp'))
        self.streams = {e: [] for e in self.engs}


def build_program(NL=2, NE=32, phases="0ABCF", dbg=False):
    nc = bass.Bass("TRN2", target_bir_lowering=False)
    P = Prog(nc)
    def din(name, shape, dt=F32):
        return nc.dram_tensor(name, list(shape), dt, kind="ExternalInput").ap()
    def dscr(name, shape, dt=BF16):
        return nc.dram_tensor(name, list(shape), dt, kind="ExternalOutput" if dbg else "Internal").ap()
    x_in = din("x", [S, D]); cT_in = din("cT", [128, 16]); pos_in = din("pos", [1, S], I32)
    w_mod = din("w_mod", [NL, D, 6 * D]); bmodT = din("bmodT", [NL, 128, 96]); bmodgt = din("bmodgt", [NL, 2, 128, D])
    g1T = din("g1T", [NL, 128, 16]); g2T = din("g2T", [NL, 128, 16])
    w_in = din("w_in", [NL, D, DIN]); gkvT = din("gkvT", [NL, 128, 4]); w_kv = din("w_kv_up", [NL, 512, 2048])
    w_pa = din("w_proj_a", [NL, 1024, D]); w_pb = din("w_proj_b", [NL, 512, D]); w_o = din("w_out", [NL, D, D])
    w_r = din("w_router", [NL, D, 32]); brow = din("brow", [NL, 128, 32])
    w_m1 = din("w_moe1", [NL, NE, D, 2 * D]); b1T = din("b1T", [NL, NE, 128, 32])
    w_m2 = din("w_moe2", [NL, NE, D, D]); b2 = din("b_moe2", [NL, 32, D])
    gfin = din("gfin", [128, D])
    c_identf = din("identf", [128, 128]); c_identb = din("identb", [128, 128], BF16)
    c_sw128 = din("sw128", [128, 128], BF16); c_sw64 = din("sw64", [128, 128], BF16)
    c_tri = din("tri01", [128, 128]); c_atri = din("atri01", [128, 128])
    c_tab = din("dtab", [128, 42, 128], BF16); c_vec = din("cvec", [128, 8]); c_pow = din("pow2", [128, 24])
    out = nc.dram_tensor("out", [S, D], F32, kind="ExternalOutput").ap()
    QA = dscr("QA", [8, 128, S]); KA = dscr("KA", [8, 128, S]); VA = dscr("VA", [S, 1024])
    QI = dscr("QI", [8, 128, S]); KI = dscr("KI", [128, S]); WI = dscr("WI", [S, 16], F32)
    QB = dscr("QB", [12, 128, S]); KB = dscr("KB", [12, 128, S]); VB = dscr("VB", [S, 1536])
    GT = dscr("GT", [32, 128, S]); X1 = dscr("X1", [S, D], F32); XA = dscr("XA", [S, D], F32)
    NFAST = min(NE, 8); NEG_ = 1
    WB1g = [nc.dram_tensor(f"WB1_{i}", [8, 8, 128, 16 * 512], BF16, kind="Internal").ap() for i in range(NEG_)]
    WB2g = [nc.dram_tensor(f"WB2_{i}", [8, 4, 128, 16 * 512], BF16, kind="Internal").ap() for i in range(NEG_)]

    es = ExitStack()
    P.es = es
    with es:
        def sb(name, shape, dt=F32):
            return es.enter_context(nc.sbuf_tensor("s_" + name, list(shape), dt))
        ps = [es.enter_context(nc.psum_tensor(f"ps{i}", [128, 512], F32)) for i in range(7)]
        psb16 = es.enter_context(nc.psum_tensor("psb16", [128, 1024], BF16))
        psk = [('ps', i) for i in range(8)]
        identf = sb("identf", [128, 128]); identb = sb("identb", [128, 128], BF16)
        sw128 = sb("sw128", [128, 128], BF16); sw64 = sb("sw64", [128, 128], BF16)
        tri01 = sb("tri01", [128, 128]); atri01 = sb("atri01", [128, 128])
        cvec = sb("cvec", [128, 8]); pow2 = sb("pow2", [128, 24])
        onesf = sb("onesf", [128, 128]); onesb = sb("onesb", [128, 128], BF16)
        modT = sb("modT", [128, 96]); G1 = sb("G1", [128, 16]); G2 = sb("G2", [128, 16])
        gtrow = sb("gtrow", [128, 2, D])
        for t, src in ((identf, c_identf), (identb, c_identb), (sw128, c_sw128), (sw64, c_sw64), (tri01, c_tri),
                       (atri01, c_atri), (cvec, c_vec), (pow2, c_pow)):
            P.op('sp', lambda t=t, src=src: nc.sync.dma_start(out=t[:], in_=src[:]), writes=[t.name], dma=True)
        P.op('dve', lambda: nc.vector.memset(onesf[:], 1.0), writes=['onesf'])
        P.op('dve', lambda: nc.vector.memset(onesb[:], 1.0), writes=['onesb'])
        P.flush()

        ctr = [0]
        def rot(n):
            ctr[0] += 1
            return ctr[0] % n

        def norm_group(es2, src, r0, ntile, Gv, SHv, hT, tagp):
            xt = es2['xt']; st = es2['st']
            for j in range(ntile):
                P.op('sp', lambda j=j: nc.sync.dma_start(out=xt[:, j, :], in_=src[r0 + j * 128:r0 + (j + 1) * 128, :]), writes=[('xt', j)], dma=True)
                P.op('act', lambda j=j: nc.scalar.activation(out=es2['junk'][:], in_=xt[:, j, :], func=AF.Square, accum_out=st[:, j:j + 1]), reads=[('xt', j)], writes=['junk', ('st', j)])
                P.op('act', lambda j=j: nc.scalar.activation(out=st[:, 8 + j:9 + j], in_=st[:, j:j + 1], func=AF.Sqrt, scale=1.0 / D, bias=cvec[:, 3:4]), reads=[('st', j)], writes=[('st2', j)])
                P.op('dve', lambda j=j: nc.vector.reciprocal(out=st[:, 16 + j:17 + j], in_=st[:, 8 + j:9 + j]), reads=[('st2', j)], writes=[('st3', j)])
                P.op('dve', lambda j=j: nc.vector.tensor_scalar(out=xt[:, j, :], in0=xt[:, j, :], scalar1=st[:, 16 + j:17 + j], scalar2=None, op0=ALU.mult), reads=[('st3', j), ('xt', j)], writes=[('xt', j)])
            for c in range(16):
                b = 5 + (c % 2)
                for j in range(ntile):
                    P.op('pe', lambda c=c, j=j, b=b: nc.tensor.transpose(out=ps[b][:, j * 128:(j + 1) * 128], in_=xt[:, j, c * 128:(c + 1) * 128], identity=identf[:]),
                         reads=[('xt', j), 'identf'], writes=[psk[b]])
                P.op('act', lambda c=c, b=b: nc.scalar.activation(out=hT[:, c, 0:128 * ntile], in_=ps[b][:, 0:128 * ntile], func=AF.Identity, scale=Gv[:, c:c + 1], bias=SHv[:, c:c + 1]),
                     reads=[psk[b], 'modT', 'G'], writes=[(tagp, c)])

        for l in range(NL):
            xsrc = x_in if l == 0 else XA
            xdst = XA if l == 0 else XA
            if '0' in phases:
                with ExitStack() as ph:
                    def sbp(name, shape, dt=F32):
                        return ph.enter_context(nc.sbuf_tensor(f"p{l}_" + name, list(shape), dt))
                    cact = sbp("cact", [128, 16]); cbc = sbp("cbc", [128, 16, 128]); c2 = sbp("c2", [128, 16, 2])
                    wm = sbp("wm", [128, 2, 16, 512]); bgt = sbp("bgt", [128, 2, D]); bmT = sbp("bmT", [128, 96]); gg = sbp("gg", [128, 32])
                    P.op('sp', lambda: nc.sync.dma_start(out=cact[:], in_=cT_in[:]), writes=['cact'], dma=True)
                    P.op('sp', lambda: nc.sync.dma_start(out=bgt[:], in_=bmodgt[l].rearrange("a p d -> p a d")), writes=['bgt'], dma=True)
                    P.op('sp', lambda: nc.sync.dma_start(out=bmT[:], in_=bmodT[l]), writes=['bmT'], dma=True)
                    P.op('sp', lambda: nc.sync.dma_start(out=gg[:, 0:16], in_=g1T[l]), writes=['gg1'], dma=True)
                    P.op('sp', lambda: nc.sync.dma_start(out=gg[:, 16:32], in_=g2T[l]), writes=['gg2'], dma=True)
                    P.op('act', lambda: nc.scalar.activation(out=cact[:], in_=cact[:], func=AF.Silu), reads=['cact'], writes=['cact'])
                    for k in range(16):
                        P.op('dve', lambda k=k: nc.vector.tensor_scalar(out=cbc[:, k, :], in0=onesf[:], scalar1=cact[:, k:k + 1], scalar2=None, op0=ALU.mult), reads=['cact', 'onesf'], writes=['cbc'])
                    for q in range(2):
                        P.op('dve', lambda q=q: nc.vector.tensor_copy(out=c2[:, :, q], in_=cact[:]), reads=['cact'], writes=['c2'])
                    col = 0
                    for n in range(24):
                        wb = n % 2
                        P.op('sp', lambda n=n, wb=wb: nc.sync.dma_start(out=wm[:, wb], in_=w_mod[l].rearrange("(c p) n -> p c n", p=128)[:, :, n * 512:(n + 1) * 512]), writes=[('wm', wb)], dma=True)
                        isgt = (8 <= n < 12) or (20 <= n < 24)
                        if isgt:
                            a = 0 if n < 12 else 1; jj = (n - 8) if n < 12 else (n - 20)
                            b = n % 2
                            for k in range(16):
                                P.op('pe', lambda k=k, wb=wb, b=b: nc.tensor.matmul(ps[b][:], lhsT=cbc[:, k, :], rhs=wm[:, wb, k, :], start=(k == 0), stop=(k == 15)), reads=['cbc', ('wm', wb)], writes=[psk[b]])
                            P.op('dve', lambda a=a, jj=jj, b=b: nc.vector.tensor_tensor(out=gtrow[:, a, jj * 512:(jj + 1) * 512], in0=ps[b][:], in1=bgt[:, a, jj * 512:(jj + 1) * 512], op=ALU.add), reads=[psk[b], 'bgt'], writes=['gtrow'])
                        else:
                            for sub in range(4):
                                ch = n * 4 + sub
                                for k in range(16):
                                    P.op('pe', lambda k=k, wb=wb, sub=sub, ch=ch: nc.tensor.matmul(ps[2][:, 2 * ch:2 * ch + 2], lhsT=wm[:, wb, k, sub * 128:(sub + 1) * 128], rhs=c2[:, k, :], start=(k == 0), stop=(k == 15)), reads=['c2', ('wm', wb)], writes=[psk[2]])
                    P.op('dve', lambda: nc.vector.memset(modT[:], 0.0), writes=['modT'])
                    for (c0, c1) in ((0, 32), (48, 80)):
                        P.op('dve', lambda c0=c0, c1=c1: nc.vector.tensor_tensor(out=modT[:, c0:c1], in0=ps[2][:, 2 * c0:2 * c1:2], in1=bmT[:, c0:c1], op=ALU.add), reads=[psk[2], 'bmT', 'modT'], writes=['modT'])
                    P.op('dve', lambda: nc.vector.scalar_tensor_tensor(out=G1[:], in0=modT[:, 16:32], scalar=1.0, in1=gg[:, 0:16], op0=ALU.add, op1=ALU.mult), reads=['modT', 'gg1'], writes=['G'])
                    P.op('dve', lambda: nc.vector.scalar_tensor_tensor(out=G2[:], in0=modT[:, 64:80], scalar=1.0, in1=gg[:, 16:32], op0=ALU.add, op1=ALU.mult), reads=['modT', 'gg2', 'G'], writes=['G'])
                    P.flush()
            if 'A' in phases:
                with ExitStack() as ph:
                    def sbp(name, shape, dt=F32):
                        return ph.enter_context(nc.sbuf_tensor(f"p{l}_" + name, list(shape), dt))
                    es2 = dict(xt=sbp("xt", [128, 4, D]), st=sbp("st", [128, 24]), junk=sbp("junk", [128, D], BF16))
                    hT = sbp("hT", [128, 16, 512], BF16)
                    wt = sbp("wt", [128, 2, 16, 512], BF16); wt80 = sbp("wt80", [128, 16, 80], BF16)
                    wkv = sbp("wkv", [128, 4, 2048], BF16); gkv = sbp("gkv", [128, 4])
                    ckr = sbp("ckr", [128, 4, 512]); cksq = sbp("cksq", [128, 4, 512], BF16); ckn = sbp("ckn", [128, 4, 512], BF16)
                    rbc = sbp("rbc", [128, 512])
                    posi = sbp("posi", [128, 512], I32); ang = sbp("ang", [128, 4, 512])
                    tabs = sbp("tabs", [128, 4, 512])
                    qbf = sbp("qbf", [128, 2, 512], BF16); t1 = sbp("t1", [128, 2, 512]); t2 = sbp("t2", [128, 2, 512])
                    osb = sbp("osb", [128, 4, 512], BF16); wisb = sbp("wisb", [128, 2, 16])
                    P.op('pool', lambda: nc.gpsimd.dma_start(out=wkv[:], in_=w_kv[l].rearrange("(c p) n -> p c n", p=128)), writes=['wkv'], dma=True)
                    P.op('sp', lambda: nc.sync.dma_start(out=gkv[:], in_=gkvT[l]), writes=['gkv'], dma=True)
                    P.op('pool', lambda: nc.gpsimd.dma_start(out=wt80[:], in_=w_in[l].rearrange("(c p) n -> p c n", p=128)[:, :, 2560:2640]), writes=['wt80'], dma=True)

                    def rope_store(psb, kind, dst_ap, extra_reads=()):
                        r = rot(2); o = rot(4)
                        if RS == 0: return
                        ci, si, swm = (0, 1, sw128) if kind == 128 else (2, 3, sw64)
                        P.op('act', lambda: nc.scalar.copy(out=qbf[:, r, :], in_=ps[psb][:]), reads=[psk[psb]], writes=[('qbf', r)])
                        if RS == 1: return
                        pb2 = 2 + r
                        P.op('pe', lambda: nc.tensor.matmul(ps[pb2][:], lhsT=swm[:], rhs=qbf[:, r, :], start=True, stop=True), reads=[('qbf', r), 'sw'], writes=[psk[pb2]])
                        if RS == 2: return
                        P.op('dve', lambda: nc.vector.tensor_tensor(out=t1[:, r, :], in0=ps[psb][:], in1=tabs[:, ci, :], op=ALU.mult), reads=[psk[psb], 'tabs'], writes=[('t1', r)])
                        P.op('dve', lambda: nc.vector.tensor_tensor(out=t2[:, r, :], in0=ps[pb2][:], in1=tabs[:, si, :], op=ALU.mult), reads=[psk[pb2], 'tabs'], writes=[('t2', r)])
                        if RS == 3: return
                        P.op('pool', lambda: nc.gpsimd.tensor_tensor(out=osb[:, o, :], in0=t1[:, r, :], in1=t2[:, r, :], op=ALU.add), reads=[('t1', r), ('t2', r)], writes=[('osb', o)])
                        P.op('sp', lambda: nc.sync.dma_start(out=dst_ap, in_=osb[:, o, :]), reads=[('osb', o)], writes=[dst_ap.tensor.name], dma=True)

                    for g in range(NGRP):
                        t0 = g * 512
                        norm_group(es2, xsrc, t0, 4, G1, modT[:, 0:16], hT, 'hT')
                        if CUT == 1: continue
                        P.op('sp', lambda t0=t0: nc.sync.dma_start(out=posi[:], in_=pos_in[0:1, t0:t0 + 512].partition_broadcast(128)), writes=['posi'], dma=True)
                        P.op('dve', lambda: nc.vector.tensor_copy(out=ang[:, 3, :], in_=posi[:]), reads=['posi'], writes=[('ang', 3)])
                        for kind_i, fcol in ((0, 0), (1, 1)):
                            sgn = cvec[:, 2:3]
                            def mkang(kind_i=kind_i, fcol=fcol):
                                a = ang[:, 0, :]; kf = ang[:, 1, :]; ki = posi
                                P.op('dve', lambda: nc.vector.tensor_scalar(out=a, in0=ang[:, 3, :], scalar1=cvec[:, fcol:fcol + 1], scalar2=None, op0=ALU.mult), reads=[('ang', 3), 'cvec'], writes=[('ang', 0)])
                                for which in (0, 1):
                                    if which == 1:
                                        P.op('dve', lambda: nc.vector.tensor_scalar(out=a, in0=a, scalar1=float(np.pi / 2), scalar2=None, op0=ALU.add), reads=[('ang', 0)], writes=[('ang', 0)])
                                    P.op('dve', lambda: nc.vector.tensor_scalar(out=kf, in0=a, scalar1=float(1 / (2 * np.pi)), scalar2=None, op0=ALU.mult), reads=[('ang', 0)], writes=[('ang', 1)])
                                    P.op('dve', lambda: nc.vector.tensor_copy(out=ki[:], in_=kf), reads=[('ang', 1)], writes=['posi'])
                                    P.op('dve', lambda: nc.vector.tensor_copy(out=kf, in_=ki[:]), reads=['posi'], writes=[('ang', 1)])
                                    r_ = ang[:, 2, :]
                                    P.op('dve', lambda: nc.vector.scalar_tensor_tensor(out=r_, in0=kf, scalar=-6.28125, in1=a, op0=ALU.mult, op1=ALU.add), reads=[('ang', 0), ('ang', 1)], writes=[('ang', 2)])
                                    P.op('dve', lambda: nc.vector.scalar_tensor_tensor(out=r_, in0=kf, scalar=-0.0019353071795864769, in1=r_, op0=ALU.mult, op1=ALU.add), reads=[('ang', 1), ('ang', 2)], writes=[('ang', 2)])
                                    P.op('dve', lambda: nc.vector.tensor_scalar(out=kf, in0=r_, scalar1=float(np.pi), scalar2=float(-2 * np.pi), op0=ALU.is_gt, op1=ALU.mult), reads=[('ang', 2)], writes=[('ang', 1)])
                                    P.op('dve', lambda: nc.vector.tensor_tensor(out=r_, in0=r_, in1=kf, op=ALU.add), reads=[('ang', 1), ('ang', 2)], writes=[('ang', 2)])
                                    P.op('dve', lambda: nc.vector.tensor_scalar(out=kf, in0=r_, scalar1=float(-np.pi), scalar2=float(2 * np.pi), op0=ALU.is_lt, op1=ALU.mult), reads=[('ang', 2)], writes=[('ang', 1)])
                                    P.op('dve', lambda: nc.vector.tensor_tensor(out=r_, in0=r_, in1=kf, op=ALU.add), reads=[('ang', 1), ('ang', 2)], writes=[('ang', 2)])
                                    P.op('dve', lambda: nc.vector.tensor_scalar(out=r_, in0=r_, scalar1=3.1415925, scalar2=-3.1415925, op0=ALU.min, op1=ALU.max), reads=[('ang', 2)], writes=[('ang', 2)])
                                    ti = 2 * kind_i + (1 - which)
                                    P.op('act', lambda ti=ti: nc.scalar.activation(out=tabs[:, ti, :], in_=r_, func=AF.Sin), reads=[('ang', 2)], writes=['tabs'])
                                    if which == 0:
                                        P.op('dve', lambda ti=ti: nc.vector.tensor_scalar(out=tabs[:, ti, :], in0=tabs[:, ti, :], scalar1=cvec[:, 2 + 2 * kind_i:3 + 2 * kind_i], scalar2=None, op0=ALU.mult), reads=['tabs', 'cvec'], writes=['tabs'])
                            mkang()
                        if CUT == 2: continue
                        def load_w(c0):
                            wb = rot(2)
                            P.op('pool', lambda: nc.gpsimd.dma_start(out=wt[:, wb], in_=w_in[l].rearrange("(c p) n -> p c n", p=128)[:, :, c0:c0 + 512]), writes=[('wt', wb)], dma=True)
                            return wb
                        def proj_fm(wb, sub, psb, wtile=None):
                            for k in range(16):
                                P.op('pe', lambda k=k: nc.tensor.matmul(ps[psb][:], lhsT=wt[:, wb, k, sub * 128:(sub + 1) * 128], rhs=hT[:, k, :], start=(k == 0), stop=(k == 15)),
                                     reads=[('wt', wb)] + [('hT', c) for c in range(16)], writes=[psk[psb]])
                        hreads = [('hT', c) for c in range(16)]
                        for cg in range(2):
                            wb = load_w(cg * 512)
                            for sub in range(4):
                                pb = rot(2); proj_fm(wb, sub, pb)
                                rope_store(pb, 128, QA[cg * 4 + sub][:, t0:t0 + 512])
                        if CUT == 3: continue
                        wb = load_w(1024)
                        for sub in range(4):
                            pb = rot(2); proj_fm(wb, sub, pb)
                            P.op('act', lambda sub=sub, pb=pb: nc.scalar.copy(out=ckr[:, sub, :], in_=ps[pb][:]), reads=[psk[pb]], writes=[('ckr', sub)])
                            P.op('act', lambda sub=sub: nc.scalar.activation(out=cksq[:, sub, :], in_=ckr[:, sub, :], func=AF.Square), reads=[('ckr', sub)], writes=[('cksq', sub)])
                        pb = rot(2)
                        for sub in range(4):
                            P.op('pe', lambda sub=sub, pb=pb: nc.tensor.matmul(ps[pb][:], lhsT=onesb[:], rhs=cksq[:, sub, :], start=(sub == 0), stop=(sub == 3)), reads=[('cksq', sub), 'onesb'], writes=[psk[pb]])
                        P.op('act', lambda pb=pb: nc.scalar.activation(out=rbc[:], in_=ps[pb][:], func=AF.Sqrt, scale=1.0 / 512, bias=cvec[:, 3:4]), reads=[psk[pb], 'cvec'], writes=['rbc'])
                        P.op('dve', lambda: nc.vector.reciprocal(out=rbc[:], in_=rbc[:]), reads=['rbc'], writes=['rbc'])
                        for sub in range(4):
                            P.op('dve', lambda sub=sub: nc.vector.scalar_tensor_tensor(out=ckn[:, sub, :], in0=ckr[:, sub, :], scalar=gkv[:, sub:sub + 1], in1=rbc[:], op0=ALU.mult, op1=ALU.mult), reads=[('ckr', sub), 'gkv', 'rbc'], writes=[('ckn', sub)])
                        cknr = [('ckn', s_) for s_ in range(4)]
                        for h in range(8):
                            pb = rot(2)
                            for k in range(4):
                                P.op('pe', lambda k=k, h=h, pb=pb: nc.tensor.matmul(ps[pb][:], lhsT=wkv[:, k, h * 256:h * 256 + 128], rhs=ckn[:, k, :], start=(k == 0), stop=(k == 3)), reads=cknr + ['wkv'], writes=[psk[pb]])
                            rope_store(pb, 128, KA[h][:, t0:t0 + 512])
                        wkv_v = [wkv[:, k, :].rearrange("p (h two d) -> p h two d", two=2, d=128) for k in range(4)]
                        for j in range(4):
                            for hh in range(2):
                                pb = rot(2); o = rot(4)
                                for k in range(4):
                                    P.op('pe', lambda k=k, j=j, hh=hh, pb=pb: nc.tensor.matmul(ps[pb][:].rearrange("p (h d) -> p h d", d=128), lhsT=ckn[:, k, j * 128:(j + 1) * 128], rhs=wkv_v[k][:, hh * 4:hh * 4 + 4, 1, :], start=(k == 0), stop=(k == 3)), reads=cknr + ['wkv'], writes=[psk[pb]])
                                P.op('act', lambda pb=pb, o=o: nc.scalar.copy(out=osb[:, o, :], in_=ps[pb][:]), reads=[psk[pb]], writes=[('osb', o)])
                                P.op('sp', lambda j=j, hh=hh, o=o, t0=t0: nc.sync.dma_start(out=VA[t0 + j * 128:t0 + (j + 1) * 128, hh * 512:(hh + 1) * 512], in_=osb[:, o, :]), reads=[('osb', o)], writes=['VA'], dma=True)
                        if CUT == 4: continue
                        for cg in range(2):
                            wb = load_w(1536 + cg * 512)
                            for sub in range(4):
                                pb = rot(2); proj_fm(wb, sub, pb)
                                rope_store(pb, 64, QI[cg * 4 + sub][:, t0:t0 + 512])
                        pb = rot(2)
                        for half in range(2):
                            for k in range(16):
                                P.op('pe', lambda k=k, half=half, pb=pb: nc.tensor.matmul(ps[pb][half * 64:(half + 1) * 64, :], lhsT=wt80[:, k, 0:64], rhs=hT[:, k, :], start=(k == 0), stop=(k == 15)), reads=hreads + ['wt80'], writes=[psk[pb]])
                        rope_store(pb, 64, KI[:, t0:t0 + 512])
                        for j in range(4):
                            pb = rot(2); o = rot(2)
                            for k in range(16):
                                P.op('pe', lambda k=k, j=j, pb=pb: nc.tensor.matmul(ps[pb][:, 0:16], lhsT=hT[:, k, j * 128:(j + 1) * 128], rhs=wt80[:, k, 64:80], start=(k == 0), stop=(k == 15)), reads=hreads + ['wt80'], writes=[psk[pb]])
                            P.op('act', lambda pb=pb, o=o: nc.scalar.mul(out=wisb[:, o, :], in_=ps[pb][:, 0:16], mul=float((64 ** -0.5) * (16 ** -0.5))), reads=[psk[pb]], writes=[('wisb', o)])
                            P.op('sp', lambda j=j, o=o, t0=t0: nc.sync.dma_start(out=WI[t0 + j * 128:t0 + (j + 1) * 128, :], in_=wisb[:, o, :]), reads=[('wisb', o)], writes=['WI'], dma=True)
                        if CUT == 5: continue
                        for which, dst in ((0, QB), (1, KB)):
                            for cg in range(3):
                                wb = load_w(2640 + which * 1536 + cg * 512)
                                for sub in range(4):
                                    pb = rot(2); proj_fm(wb, sub, pb)
                                    rope_store(pb, 128, dst[cg * 4 + sub][:, t0:t0 + 512])
                        for cg in range(3):
                            wb = load_w(5712 + cg * 512)
                            for j in range(4):
                                pb = rot(2); o = rot(4)
                                for k in range(16):
                                    P.op('pe', lambda k=k, j=j, pb=pb, wb=wb: nc.tensor.matmul(ps[pb][:], lhsT=hT[:, k, j * 128:(j + 1) * 128], rhs=wt[:, wb, k, :], start=(k == 0), stop=(k == 15)), reads=hreads + [('wt', wb)], writes=[psk[pb]])
                                P.op('act', lambda pb=pb, o=o: nc.scalar.copy(out=osb[:, o, :], in_=ps[pb][:]), reads=[psk[pb]], writes=[('osb', o)])
                                P.op('sp', lambda j=j, cg=cg, o=o, t0=t0: nc.sync.dma_start(out=VB[t0 + j * 128:t0 + (j + 1) * 128, cg * 512:(cg + 1) * 512], in_=osb[:, o, :]), reads=[('osb', o)], writes=['VB'], dma=True)
                        for cg in range(8):
                            wb = load_w(7248 + cg * 512)
                            for sub in range(4):
                                pb = rot(2); o = rot(4); proj_fm(wb, sub, pb)
                                P.op('act', lambda pb=pb, o=o: nc.scalar.activation(out=osb[:, o, :], in_=ps[pb][:], func=AF.Sigmoid), reads=[psk[pb]], writes=[('osb', o)])
                                P.op('sp', lambda cg=cg, sub=sub, o=o, t0=t0: nc.sync.dma_start(out=GT[cg * 4 + sub][:, t0:t0 + 512], in_=osb[:, o, :]), reads=[('osb', o)], writes=['GT'], dma=True)
                    P.flush()

            if 'B' in phases:
                with ExitStack() as phB:
                    def sbB(name, shape, dt=F32):
                        return phB.enter_context(nc.sbuf_tensor(f"b{l}_" + name, list(shape), dt))
                    dtab = sbB("dtab", [128, 42, 128], BF16)
                    mbT = sbB("mbT", [128, 32, 512], BF16)
                    oa = sbB("oa", [128, 8, 512], BF16); ob = sbB("ob", [128, 4, 512], BF16)
                    P.op('sp', lambda: nc.sync.dma_start(out=dtab[:], in_=c_tab[:]), writes=['dtab'], dma=True)
                    for G in range(NGRP):
                        q0 = G * 512; nkb = 4 * G + 4; nk = nkb * 128
                        with ExitStack() as ph:
                            def sbp(name, shape, dt=F32):
                                return ph.enter_context(nc.sbuf_tensor(f"ba{l}_{G}_" + name, list(shape), dt))
                            sc = sbp("sc", [128, S]); junkb = sbp("junkb", [128, S], BF16); mb01 = sbp("mb01", [128, 2, S], BF16)
                            qi_sb = sbp("qi", [128, 8, 512], BF16); ki_sb = sbp("ki", [128, S], BF16); wi_sb = sbp("wi", [128, 4, 16])
                            rl = sbp("rl", [128, 3, 512]); bs = sbp("bs", [128, 64]); dg = sbp("dg", [128, 128])
                            P.op('sp', lambda: nc.sync.dma_start(out=qi_sb[:], in_=QI[:, :, q0:q0 + 512].rearrange("c p t -> p c t")), reads=['QI'], writes=['qi_sb'], dma=True)
                            P.op('sp', lambda: nc.sync.dma_start(out=ki_sb[:, 0:nk], in_=KI[:, 0:nk]), reads=['KI'], writes=['ki_sb'], dma=True)
                            P.op('sp', lambda: nc.sync.dma_start(out=wi_sb[:], in_=WI[q0:q0 + 512, :].rearrange("(j p) h -> p j h", p=128)), reads=['WI'], writes=['wi_sb'], dma=True)
                            for j in range(4):
                                nkj = 128 * (4 * G + j + 1); jb = j % 2
                                nch = (nkj + 511) // 512
                                for ch in range(nch):
                                    kc = ch * 512; w = min(512, nkj - kc)
                                    for h in range(16):
                                        pb = rot(2); r3 = rot(3); hf = (h % 2) * 64
                                        P.op('pe', lambda h=h, pb=pb, hf=hf, kc=kc, w=w, j=j: nc.tensor.matmul(ps[pb][:, 0:w], lhsT=qi_sb[hf:hf + 64, h // 2, j * 128:(j + 1) * 128], rhs=ki_sb[hf:hf + 64, kc:kc + w], start=True, stop=True),
                                             reads=['qi_sb', 'ki_sb'], writes=[psk[pb]])
                                        P.op('act', lambda pb=pb, r3=r3, w=w: nc.scalar.activation(out=rl[:, r3, 0:w], in_=ps[pb][:, 0:w], func=AF.Relu), reads=[psk[pb]], writes=[('rl', r3)])
                                        if h == 0:
                                            P.op('dve', lambda r3=r3, kc=kc, w=w, j=j: nc.vector.tensor_scalar(out=sc[:, kc:kc + w], in0=rl[:, r3, 0:w], scalar1=wi_sb[:, j, 0:1], scalar2=None, op0=ALU.mult), reads=[('rl', r3), 'wi_sb'], writes=[('sc', ch)])
                                        else:
                                            P.op('dve', lambda r3=r3, kc=kc, w=w, j=j, h=h: nc.vector.scalar_tensor_tensor(out=sc[:, kc:kc + w], in0=rl[:, r3, 0:w], scalar=wi_sb[:, j, h:h + 1], in1=sc[:, kc:kc + w], op0=ALU.mult, op1=ALU.add), reads=[('rl', r3), 'wi_sb', ('sc', ch)], writes=[('sc', ch)])
                                scall = [('sc', ch) for ch in range(nch)]
                                V = lambda fn, rd, wr: P.op('dve', fn, reads=rd, writes=wr)
                                V(lambda nkj=nkj: nc.vector.tensor_reduce(out=bs[:, 0:1], in_=sc[:, 0:nkj], axis=AX.X, op=ALU.min), scall, ['bs'])
                                V(lambda nkj=nkj: nc.vector.tensor_reduce(out=bs[:, 1:2], in_=sc[:, 0:nkj], axis=AX.X, op=ALU.max), scall + ['bs'], ['bs'])
                                V(lambda: nc.vector.tensor_scalar(out=bs[:, 2:3], in0=bs[:, 0:1], scalar1=-1.0, scalar2=None, op0=ALU.add), ['bs'], ['bs'])
                                V(lambda: nc.vector.tensor_scalar(out=bs[:, 3:4], in0=bs[:, 0:1], scalar1=-0.5, scalar2=None, op0=ALU.add), ['bs'], ['bs'])
                                V(lambda: nc.vector.tensor_tensor(out=bs[:, 4:5], in0=bs[:, 1:2], in1=bs[:, 3:4], op=ALU.subtract), ['bs'], ['bs'])
                                V(lambda: nc.vector.tensor_scalar(out=bs[:, 8:32], in0=pow2[:], scalar1=bs[:, 4:5], scalar2=0.5, op0=ALU.mult, op1=ALU.mult), ['bs', 'pow2'], ['bs'])
                                V(lambda: nc.vector.tensor_tensor(out=bs[:, 5:6], in0=bs[:, 3:4], in1=bs[:, 8:9], op=ALU.add), ['bs'], ['bs'])
                                dk = nkj - 128; dch = dk // 512
                                V(lambda dk=dk: nc.vector.tensor_tensor(out=dg[:], in0=sc[:, dk:dk + 128], in1=tri01[:], op=ALU.mult), scall + ['tri01'], ['dg'])
                                V(lambda dk=dk: nc.vector.scalar_tensor_tensor(out=sc[:, dk:dk + 128], in0=atri01[:], scalar=bs[:, 2:3], in1=dg[:], op0=ALU.mult, op1=ALU.add), ['dg', 'bs', 'atri01'], [('sc', dch)])
                                for it in range(24):
                                    V(lambda nkj=nkj: nc.vector.tensor_scalar(out=junkb[:, 0:nkj], in0=sc[:, 0:nkj], scalar1=bs[:, 5:6], scalar2=None, op0=ALU.is_ge, op1=ALU.add, accum_out=bs[:, 6:7]), scall + ['bs'], ['junkb', 'bs'])
                                    V(lambda: nc.vector.tensor_scalar(out=bs[:, 7:8], in0=bs[:, 6:7], scalar1=255.5, scalar2=None, op0=ALU.is_ge), ['bs'], ['bs'])
                                    V(lambda it=it: nc.vector.scalar_tensor_tensor(out=bs[:, 3:4], in0=bs[:, 7:8], scalar=bs[:, 8 + it:9 + it], in1=bs[:, 3:4], op0=ALU.mult, op1=ALU.add), ['bs'], ['bs'])
                                    if it < 23:
                                        V(lambda it=it: nc.vector.tensor_tensor(out=bs[:, 5:6], in0=bs[:, 3:4], in1=bs[:, 9 + it:10 + it], op=ALU.add), ['bs'], ['bs'])
                                V(lambda nkj=nkj, jb=jb: nc.vector.tensor_scalar(out=mb01[:, jb, 0:nkj], in0=sc[:, 0:nkj], scalar1=bs[:, 3:4], scalar2=1.0, op0=ALU.is_ge, op1=ALU.subtract), scall + ['bs'], [('mb01', jb)])
                                if nkj < nk:
                                    P.op('pool', lambda nkj=nkj, jb=jb: nc.gpsimd.memset(mb01[:, jb, nkj:nk], -1.0), writes=[('mb01', jb)])
                                for kb0 in range(0, nkb, 4):
                                    nb = min(4, nkb - kb0); hb = rot(2) * 512
                                    for i in range(nb):
                                        P.op('pe', lambda i=i, kb0=kb0, jb=jb, hb=hb: nc.tensor.transpose(out=psb16[:, hb + i * 128:hb + (i + 1) * 128], in_=mb01[:, jb, (kb0 + i) * 128:(kb0 + i + 1) * 128], identity=identb[:]),
                                             reads=[('mb01', jb), 'identb'], writes=[psk[7]])
                                    P.op('act', lambda kb0=kb0, nb=nb, j=j, hb=hb: nc.scalar.mul(out=mbT[:, kb0:kb0 + nb, j * 128:(j + 1) * 128], in_=psb16[:, hb:hb + nb * 128].rearrange("p (n q) -> p n q", q=128), mul=-NEG),
                                         reads=[psk[7]], writes=[('mbT', kb0 + i) for i in range(nb)])
                            P.flush()
                        with ExitStack() as ph:
                            def sbp(name, shape, dt=F32):
                                return ph.enter_context(nc.sbuf_tensor(f"bb{l}_{G}_" + name, list(shape), dt))
                            kh = sbp("kh", [128, 2, S], BF16); vh = sbp("vh", [128, 2, 32, 128], BF16); qh = sbp("qh", [128, 2, 512], BF16)
                            pT = sbp("pT", [128, 3, 512], BF16); rinv = sbp("rinv", [128, 2, 512])
                            scale = float(128 ** -0.5)
                            def unit(kap, qap, mask_ap, vap, first, last, bo, bsum, rd, maskkeys):
                                pb = rot(2); r3 = rot(3)
                                P.op('pe', lambda: nc.tensor.matmul(ps[pb][:], lhsT=kap, rhs=qap, start=True, stop=False), reads=rd, writes=[psk[pb]])
                                P.op('pe', lambda: nc.tensor.matmul(ps[pb][:], lhsT=identb[:], rhs=mask_ap, start=False, stop=True), reads=maskkeys + ['identb'], writes=[psk[pb]])
                                P.op('act', lambda: nc.scalar.activation(out=pT[:, r3, :], in_=ps[pb][:], func=AF.Exp, scale=scale), reads=[psk[pb]], writes=[('pT', r3)])
                                P.op('pe', lambda: nc.tensor.matmul(ps[bo][:], lhsT=vap, rhs=pT[:, r3, :], start=first, stop=last), reads=rd + [('pT', r3)], writes=[psk[bo]])
                                P.op('pe', lambda: nc.tensor.matmul(ps[bsum][:], lhsT=onesb[:], rhs=pT[:, r3, :], start=first, stop=last), reads=[('pT', r3), 'onesb'], writes=[psk[bsum]])
                            def finalize(bo, bsum, dst, dkey):
                                rr_ = rot(2)
                                P.op('dve', lambda: nc.vector.reciprocal(out=rinv[:, rr_, :], in_=ps[bsum][:]), reads=[psk[bsum]], writes=[('rinv', rr_)])
                                P.op('dve', lambda: nc.vector.tensor_tensor(out=dst, in0=ps[bo][:], in1=rinv[:, rr_, :], op=ALU.mult), reads=[psk[bo], ('rinv', rr_)], writes=[dkey])
                            for h in range(8):
                                rr = h % 2
                                P.op('sp', lambda h=h, rr=rr: nc.sync.dma_start(out=qh[:, rr, :], in_=QA[h][:, q0:q0 + 512]), reads=['QA'], writes=[('qh', rr)], dma=True)
                                P.op('sp', lambda h=h, rr=rr: nc.sync.dma_start(out=kh[:, rr, 0:nk], in_=KA[h][:, 0:nk]), reads=['KA'], writes=[('kh', rr)], dma=True)
                                P.op('sp', lambda h=h, rr=rr: nc.sync.dma_start(out=vh[:, rr, 0:nkb, :], in_=VA[0:nk, h * 128:(h + 1) * 128].rearrange("(kb p) d -> p kb d", p=128)), reads=['VA'], writes=[('vh', rr)], dma=True)
                                for kb in range(nkb):
                                    unit(kh[:, rr, kb * 128:(kb + 1) * 128], qh[:, rr, :], mbT[:, kb, :], vh[:, rr, kb, :], kb == 0, kb == nkb - 1, 2 + rr, 4 + rr,
                                         [('qh', rr), ('kh', rr), ('vh', rr)], [('mbT', kb)])
                                finalize(2 + rr, 4 + rr, oa[:, h, :], ('oa', h))
                            B0 = 4 * G; toff = (0, 8, 19)
                            for hd in range(4):
                                units = []
                                for g, (W, dil) in enumerate(PAT):
                                    lo_kb = max(0, B0 - W // 128)
                                    for kb in range(lo_kb, B0 + 4):
                                        units.append((g, lo_kb, kb))
                                curg = -1
                                for ui, (g, lo_kb, kb) in enumerate(units):
                                    hh = g * 4 + hd
                                    if g != curg:
                                        curg = g; rr = rot(2)
                                        k0 = lo_kb * 128; k1 = (B0 + 4) * 128; nb_ = B0 + 4 - lo_kb
                                        P.op('sp', lambda hh=hh, rr=rr: nc.sync.dma_start(out=qh[:, rr, :], in_=QB[hh][:, q0:q0 + 512]), reads=['QB'], writes=[('qh', rr)], dma=True)
                                        P.op('sp', lambda hh=hh, rr=rr, k0=k0, k1=k1: nc.sync.dma_start(out=kh[:, rr, 0:k1 - k0], in_=KB[hh][:, k0:k1]), reads=['KB'], writes=[('kh', rr)], dma=True)
                                        P.op('sp', lambda hh=hh, rr=rr, k0=k0, k1=k1, nb_=nb_: nc.sync.dma_start(out=vh[:, rr, 0:nb_, :], in_=VB[k0:k1, hh * 128:(hh + 1) * 128].rearrange("(kb p) d -> p kb d", p=128)), reads=['VB'], writes=[('vh', rr)], dma=True)
                                    i = kb - lo_kb; d0 = B0 - kb; ti = toff[g] + d0 + 3
                                    unit(kh[:, rr, i * 128:(i + 1) * 128], qh[:, rr, :], dtab[:, ti:ti + 4, :].rearrange("p n q -> p (n q)"), vh[:, rr, i, :], ui == 0, ui == len(units) - 1, 2 + hd % 2, 4 + hd % 2,
                                         [('qh', rr), ('kh', rr), ('vh', rr)], ['dtab'])
                                finalize(2 + hd % 2, 4 + hd % 2, ob[:, hd, :], ('ob', hd))
                            P.flush()
                        with ExitStack() as ph:
                            def sbp(name, shape, dt=F32):
                                return ph.enter_context(nc.sbuf_tensor(f"bc{l}_{G}_" + name, list(shape), dt))
                            gsb = sbp("gsb", [128, 2, 2, 512], BF16); wpa = sbp("wpa", [128, 2, 8, 512], BF16); wpb = sbp("wpb", [128, 2, 4, 512], BF16)
                            mg = sbp("mg", [128, 16, 512], BF16); tm = sbp("tm", [128, 2, 2, 512]); wo = sbp("wo", [128, 2, 16, 512], BF16)
                            xr = sbp("xr", [128, 2, 512]); xo = sbp("xo", [128, 2, 512])
                            oar = [('oa', h) for h in range(8)]; obr = [('ob', h) for h in range(4)]
                            for cg in range(4):
                                w_ = cg % 2
                                P.op('pool', lambda cg=cg, w_=w_: nc.gpsimd.dma_start(out=wpa[:, w_], in_=w_pa[l].rearrange("(c p) n -> p c n", p=128)[:, :, cg * 512:(cg + 1) * 512]), writes=[('wpa', w_)], dma=True)
                                P.op('pool', lambda cg=cg, w_=w_: nc.gpsimd.dma_start(out=wpb[:, w_], in_=w_pb[l].rearrange("(c p) n -> p c n", p=128)[:, :, cg * 512:(cg + 1) * 512]), writes=[('wpb', w_)], dma=True)
                                for sub in range(4):
                                    c = cg * 4 + sub; r = c % 2; pa_ = rot(2); pb_ = 2 + rot(2)
                                    P.op('sp', lambda c=c, r=r: nc.sync.dma_start(out=gsb[:, r, 0, :], in_=GT[c][:, q0:q0 + 512]), reads=['GT'], writes=[('gsb', r, 0)], dma=True)
                                    P.op('sp', lambda c=c, r=r: nc.sync.dma_start(out=gsb[:, r, 1, :], in_=GT[16 + c][:, q0:q0 + 512]), reads=['GT'], writes=[('gsb', r, 1)], dma=True)
                                    for k in range(8):
                                        P.op('pe', lambda k=k, w_=w_, sub=sub, pa_=pa_: nc.tensor.matmul(ps[pa_][:], lhsT=wpa[:, w_, k, sub * 128:(sub + 1) * 128], rhs=oa[:, k, :], start=(k == 0), stop=(k == 7)), reads=oar + [('wpa', w_)], writes=[psk[pa_]])
                                    for k in range(4):
                                        P.op('pe', lambda k=k, w_=w_, sub=sub, pb_=pb_: nc.tensor.matmul(ps[pb_][:], lhsT=wpb[:, w_, k, sub * 128:(sub + 1) * 128], rhs=ob[:, k, :], start=(k == 0), stop=(k == 3)), reads=obr + [('wpb', w_)], writes=[psk[pb_]])
                                    P.op('dve', lambda r=r, pa_=pa_: nc.vector.tensor_tensor(out=tm[:, r, 0, :], in0=ps[pa_][:], in1=gsb[:, r, 0, :], op=ALU.mult), reads=[psk[pa_], ('gsb', r, 0)], writes=[('tm', r, 0)])
                                    P.op('dve', lambda r=r, pb_=pb_: nc.vector.tensor_tensor(out=tm[:, r, 1, :], in0=ps[pb_][:], in1=gsb[:, r, 1, :], op=ALU.mult), reads=[psk[pb_], ('gsb', r, 1)], writes=[('tm', r, 1)])
                                    P.op('pool', lambda r=r, c=c: nc.gpsimd.tensor_tensor(out=mg[:, c, :], in0=tm[:, r, 0, :], in1=tm[:, r, 1, :], op=ALU.add), reads=[('tm', r, 0), ('tm', r, 1)], writes=[('mg', c)])
                            mgr = [('mg', c) for c in range(16)]
                            for cg in range(4):
                                w_ = cg % 2
                                P.op('pool', lambda cg=cg, w_=w_: nc.gpsimd.dma_start(out=wo[:, w_], in_=w_o[l].rearrange("(c p) n -> p c n", p=128)[:, :, cg * 512:(cg + 1) * 512]), writes=[('wo', w_)], dma=True)
                                for j in range(4):
                                    pb = rot(2); r = rot(2)
                                    P.op('sp', lambda cg=cg, j=j, r=r: nc.sync.dma_start(out=xr[:, r, :], in_=xsrc[q0 + j * 128:q0 + (j + 1) * 128, cg * 512:(cg + 1) * 512]), writes=[('xr', r)], dma=True)
                                    for k in range(16):
                                        P.op('pe', lambda k=k, j=j, w_=w_, pb=pb: nc.tensor.matmul(ps[pb][:], lhsT=mg[:, k, j * 128:(j + 1) * 128], rhs=wo[:, w_, k, :], start=(k == 0), stop=(k == 15)), reads=mgr + [('wo', w_)], writes=[psk[pb]])
                                    P.op('dve', lambda cg=cg, pb=pb, r=r: nc.vector.tensor_tensor(out=xo[:, r, :], in0=ps[pb][:], in1=gtrow[:, 0, cg * 512:(cg + 1) * 512], op=ALU.mult), reads=[psk[pb], 'gtrow'], writes=[('xo', r)])
                                    P.op('pool', lambda r=r: nc.gpsimd.tensor_tensor(out=xo[:, r, :], in0=xo[:, r, :], in1=xr[:, r, :], op=ALU.add), reads=[('xo', r), ('xr', r)], writes=[('xo', r)])
                                    P.op('sp', lambda cg=cg, j=j, r=r: nc.sync.dma_start(out=X1[q0 + j * 128:q0 + (j + 1) * 128, cg * 512:(cg + 1) * 512], in_=xo[:, r, :]), reads=[('xo', r)], writes=['X1'], dma=True)
                            P.flush()

            if 'C' in phases:
                with ExitStack() as ph:
                    cv = ph.enter_context(nc.sbuf_tensor(f"w{l}_cv", [128, 3, 16, 512], BF16))
                    for e in range(NFAST):
                        for cc in range(12):
                            w_ = rot(3)
                            if cc < 8:
                                src = w_m1[l, e].rearrange("(k p) n -> p k n", p=128)[:, :, cc * 512:(cc + 1) * 512]
                                dst = WB1g[0][e, cc].rearrange("p (k n) -> p k n", n=512); dk = ('WB1', e)
                            else:
                                src = w_m2[l, e].rearrange("(k p) n -> p k n", p=128)[:, :, (cc - 8) * 512:(cc - 7) * 512]
                                dst = WB2g[0][e, cc - 8].rearrange("p (k n) -> p k n", n=512); dk = ('WB2', e)
                            P.op('pool', lambda src=src, w_=w_: nc.gpsimd.dma_start(out=cv[:, w_], in_=src), writes=[('cv', w_)], dma=True)
                            P.op('sp', lambda dst=dst, w_=w_: nc.sync.dma_start(out=dst, in_=cv[:, w_]), reads=[('cv', w_)], writes=[dk], dma=True)
                    P.flush()
            if 'C' in phases:
                with ExitStack() as phC:
                    def sbC(name, shape, dt=F32):
                        return phC.enter_context(nc.sbuf_tensor(f"c{l}_" + name, list(shape), dt))
                    wr = sbC("wr", [128, 16, 32], BF16); browsb = sbC("brow", [128, 32]); b2sb = sbC("b2sb", [32, D])
                    h2T = sbC("h2T", [128, 16, 512], BF16); comb = sbC("comb", [128, 4, 32]); combT = sbC("combT", [32, 512])
                    acc = sbC("acc", [128, 4, D])
                    P.op('pool', lambda: nc.gpsimd.dma_start(out=wr[:], in_=w_r[l].rearrange("(c p) n -> p c n", p=128)), writes=['wr'], dma=True)
                    P.op('sp', lambda: nc.sync.dma_start(out=browsb[:], in_=brow[l]), writes=['browsb'], dma=True)
                    P.op('sp', lambda: nc.sync.dma_start(out=b2sb[:], in_=b2[l]), writes=['b2sb'], dma=True)
                    NPASS = NGRP
                    for p_ in range(NPASS):
                        t0 = p_ * 512
                        with ExitStack() as ph:
                            def sbp(name, shape, dt=F32):
                                return ph.enter_context(nc.sbuf_tensor(f"ca{l}_{p_}_" + name, list(shape), dt))
                            es2 = dict(xt=sbp("xt", [128, 4, D]), st=sbp("st", [128, 24]), junk=sbp("junk", [128, D], BF16))
                            rt = sbp("rt", [128, 8, 32])
                            norm_group(es2, X1, t0, 4, G2, modT[:, 48:64], h2T, 'h2T')
                            h2r = [('h2T', c) for c in range(16)]
                            for j in range(4):
                                pb = rot(2)
                                for k in range(16):
                                    P.op('pe', lambda k=k, j=j, pb=pb: nc.tensor.matmul(ps[pb][:, 0:32], lhsT=h2T[:, k, j * 128:(j + 1) * 128], rhs=wr[:, k, :], start=(k == 0), stop=(k == 15)), reads=h2r + ['wr'], writes=[psk[pb]])
                                V = lambda fn, rd, wr_: P.op('dve', fn, reads=rd, writes=wr_)
                                V(lambda pb=pb: nc.vector.tensor_tensor(out=rt[:, 0, :], in0=ps[pb][:, 0:32], in1=browsb[:], op=ALU.add), [psk[pb], 'browsb'], ['rt'])
                                V(lambda: nc.vector.max(out=rt[:, 1, 0:8], in_=rt[:, 0, :]), ['rt'], ['rt'])
                                V(lambda: nc.vector.tensor_scalar(out=rt[:, 1, 8:9], in0=rt[:, 1, 0:1], scalar1=-1.0, scalar2=None, op0=ALU.mult), ['rt'], ['rt'])
                                P.op('act', lambda: nc.scalar.activation(out=rt[:, 2, :], in_=rt[:, 0, :], func=AF.Exp, bias=rt[:, 1, 8:9]), reads=['rt'], writes=['rt'])
                                V(lambda: nc.vector.tensor_scalar(out=rt[:, 3, :], in0=rt[:, 0, :], scalar1=rt[:, 1, 3:4], scalar2=None, op0=ALU.is_ge), ['rt'], ['rt'])
                                V(lambda: nc.vector.tensor_tensor(out=rt[:, 2, :], in0=rt[:, 2, :], in1=rt[:, 3, :], op=ALU.mult), ['rt'], ['rt'])
                                V(lambda: nc.vector.tensor_reduce(out=rt[:, 1, 9:10], in_=rt[:, 2, :], axis=AX.X, op=ALU.add), ['rt'], ['rt'])
                                V(lambda: nc.vector.reciprocal(out=rt[:, 1, 10:11], in_=rt[:, 1, 9:10]), ['rt'], ['rt'])
                                V(lambda j=j: nc.vector.tensor_scalar(out=comb[:, j, :], in0=rt[:, 2, :], scalar1=rt[:, 1, 10:11], scalar2=None, op0=ALU.mult), ['rt'], [('comb', j)])
                                pb2 = 2 + rot(2)
                                P.op('pe', lambda j=j, pb2=pb2: nc.tensor.transpose(out=ps[pb2][0:32, 0:128], in_=comb[:, j, :], identity=identf[:]), reads=[('comb', j), 'identf'], writes=[psk[pb2]])
                                P.op('act', lambda j=j, pb2=pb2: nc.scalar.copy(out=combT[:, j * 128:(j + 1) * 128], in_=ps[pb2][0:32, 0:128]), reads=[psk[pb2]], writes=[('combT', j)])
                                for cg in range(4):
                                    pb3 = 4 + rot(2)
                                    P.op('pe', lambda j=j, cg=cg, pb3=pb3: nc.tensor.matmul(ps[pb3][:], lhsT=combT[:, j * 128:(j + 1) * 128], rhs=b2sb[:, cg * 512:(cg + 1) * 512], start=True, stop=True), reads=[('combT', j), 'b2sb'], writes=[psk[pb3]])
                                    P.op('act', lambda j=j, cg=cg, pb3=pb3: nc.scalar.copy(out=acc[:, j, cg * 512:(cg + 1) * 512], in_=ps[pb3][:]), reads=[psk[pb3]], writes=[('acc', j, cg)])
                            P.flush()
                        with ExitStack() as ph:
                            def sbp(name, shape, dt=F32):
                                return ph.enter_context(nc.sbuf_tensor(f"cb{l}_{p_}_" + name, list(shape), dt))
                            actT = sbp("actT", [128, 16, 512], BF16); w1t = sbp("w1t", [128, 2, 16, 512], BF16); w2t = sbp("w2t", [128, 2, 16, 512], BF16)
                            b1sb = sbp("b1sb", [128, 2, 32]); tg = sbp("tg", [128, 2, 4, 512])
                            h2r = [('h2T', c) for c in range(16)]
                            for e in range(NE):
                                er = e % 2
                                P.op('sp', lambda e=e, er=er: nc.sync.dma_start(out=b1sb[:, er, :], in_=b1T[l, e]), writes=[('b1sb', er)], dma=True)
                                for cc in range(8):
                                    w_ = rot(2)
                                    if e < NFAST:
                                        P.op('sp', lambda e=e, cc=cc, w_=w_: nc.sync.dma_start(out=w1t[:, w_], in_=WB1g[0][e, cc].rearrange("p (k n) -> p k n", n=512)), reads=[('WB1', e)], writes=[('w1t', w_)], dma=True)
                                    else:
                                        P.op('pool', lambda e=e, cc=cc, w_=w_: nc.gpsimd.dma_start(out=w1t[:, w_], in_=w_m1[l, e].rearrange("(k p) n -> p k n", p=128)[:, :, cc * 512:(cc + 1) * 512]), writes=[('w1t', w_)], dma=True)
                                    for sub in range(2):
                                        c = cc * 2 + sub; o0 = sub * 256
                                        pg = rot(2); pl = 2 + rot(2); r = rot(2)
                                        for k in range(16):
                                            P.op('pe', lambda k=k, w_=w_, pg=pg, o0=o0: nc.tensor.matmul(ps[pg][:], lhsT=w1t[:, w_, k, o0:o0 + 256:2], rhs=h2T[:, k, :], start=(k == 0), stop=(k == 15)), reads=h2r + [('w1t', w_)], writes=[psk[pg]])
                                        for k in range(16):
                                            P.op('pe', lambda k=k, w_=w_, pl=pl, o0=o0: nc.tensor.matmul(ps[pl][:], lhsT=w1t[:, w_, k, o0 + 1:o0 + 256:2], rhs=h2T[:, k, :], start=(k == 0), stop=(k == 15)), reads=h2r + [('w1t', w_)], writes=[psk[pl]])
                                        P.op('dve', lambda c=c, er=er, pg=pg, r=r: nc.vector.tensor_scalar(out=tg[:, r, 0, :], in0=ps[pg][:], scalar1=b1sb[:, er, c:c + 1], scalar2=7.0, op0=ALU.add, op1=ALU.min), reads=[psk[pg], ('b1sb', er)], writes=[('tg', r, 0)])
                                        P.op('act', lambda r=r: nc.scalar.activation(out=tg[:, r, 1, :], in_=tg[:, r, 0, :], func=AF.Sigmoid, scale=1.702), reads=[('tg', r, 0)], writes=[('tg', r, 1)])
                                        P.op('dve', lambda c=c, er=er, pl=pl, r=r: nc.vector.tensor_scalar(out=tg[:, r, 2, :], in0=ps[pl][:], scalar1=b1sb[:, er, 16 + c:17 + c], scalar2=7.0, op0=ALU.add, op1=ALU.min), reads=[psk[pl], ('b1sb', er)], writes=[('tg', r, 2)])
                                        P.op('dve', lambda r=r: nc.vector.tensor_scalar(out=tg[:, r, 2, :], in0=tg[:, r, 2, :], scalar1=-7.0, scalar2=1.0, op0=ALU.max, op1=ALU.add), reads=[('tg', r, 2)], writes=[('tg', r, 2)])
                                        P.op('dve', lambda r=r: nc.vector.tensor_tensor(out=tg[:, r, 3, :], in0=tg[:, r, 0, :], in1=tg[:, r, 1, :], op=ALU.mult), reads=[('tg', r, 0), ('tg', r, 1)], writes=[('tg', r, 3)])
                                        P.op('dve', lambda r=r, c=c: nc.vector.tensor_tensor(out=actT[:, c, :], in0=tg[:, r, 3, :], in1=tg[:, r, 2, :], op=ALU.mult), reads=[('tg', r, 3), ('tg', r, 2)], writes=[('actT', c)])
                                actr = [('actT', c) for c in range(16)]
                                for c2 in range(4):
                                    w_ = rot(2)
                                    if e < NFAST:
                                        P.op('sp', lambda e=e, c2=c2, w_=w_: nc.sync.dma_start(out=w2t[:, w_], in_=WB2g[0][e, c2].rearrange("p (k n) -> p k n", n=512)), reads=[('WB2', e)], writes=[('w2t', w_)], dma=True)
                                    else:
                                        P.op('pool', lambda e=e, c2=c2, w_=w_: nc.gpsimd.dma_start(out=w2t[:, w_], in_=w_m2[l, e].rearrange("(f p) n -> p f n", p=128)[:, :, c2 * 512:(c2 + 1) * 512]), writes=[('w2t', w_)], dma=True)
                                    for j in range(4):
                                        pb = 4 + rot(2)
                                        for f in range(16):
                                            P.op('pe', lambda f=f, j=j, w_=w_, pb=pb: nc.tensor.matmul(ps[pb][:], lhsT=actT[:, f, j * 128:(j + 1) * 128], rhs=w2t[:, w_, f, :], start=(f == 0), stop=(f == 15)), reads=actr + [('w2t', w_)], writes=[psk[pb]])
                                        P.op('dve', lambda j=j, c2=c2, e=e, pb=pb: nc.vector.scalar_tensor_tensor(out=acc[:, j, c2 * 512:(c2 + 1) * 512], in0=ps[pb][:], scalar=comb[:, j, e:e + 1], in1=acc[:, j, c2 * 512:(c2 + 1) * 512], op0=ALU.mult, op1=ALU.add),
                                             reads=[psk[pb], ('comb', j), ('acc', j, c2)], writes=[('acc', j, c2)])
                            P.flush()
                        with ExitStack() as ph:
                            def sbp(name, shape, dt=F32):
                                return ph.enter_context(nc.sbuf_tensor(f"cc{l}_{p_}_" + name, list(shape), dt))
                            xr = sbp("xr", [128, 2, D])
                            for j in range(4):
                                r = j % 2
                                P.op('sp', lambda j=j, r=r: nc.sync.dma_start(out=xr[:, r, :], in_=X1[t0 + j * 128:t0 + (j + 1) * 128, :]), reads=['X1'], writes=[('xr', r)], dma=True)
                                for cg in range(4):
                                    P.op('dve', lambda j=j, cg=cg: nc.vector.tensor_tensor(out=acc[:, j, cg * 512:(cg + 1) * 512], in0=acc[:, j, cg * 512:(cg + 1) * 512], in1=gtrow[:, 1, cg * 512:(cg + 1) * 512], op=ALU.mult), reads=[('acc', j, cg), 'gtrow'], writes=[('acc', j, cg)])
                                    P.op('pool', lambda j=j, cg=cg, r=r: nc.gpsimd.tensor_tensor(out=xr[:, r, cg * 512:(cg + 1) * 512], in0=xr[:, r, cg * 512:(cg + 1) * 512], in1=acc[:, j, cg * 512:(cg + 1) * 512], op=ALU.add), reads=[('acc', j, cg), ('xr', r)], writes=[('xr', r)])
                                P.op('sp', lambda j=j, r=r: nc.sync.dma_start(out=XA[t0 + j * 128:t0 + (j + 1) * 128, :], in_=xr[:, r, :]), reads=[('xr', r)], writes=['XA'], dma=True)
                            P.flush()
        if 'F' in phases:
            with ExitStack() as ph:
                xf = ph.enter_context(nc.sbuf_tensor("f_x", [128, 2, D], F32)); gf = ph.enter_context(nc.sbuf_tensor("f_g", [128, D], F32))
                sf = ph.enter_context(nc.sbuf_tensor("f_s", [128, 2, 4], F32)); jf = ph.enter_context(nc.sbuf_tensor("f_j", [128, D], BF16))
                P.op('sp', lambda: nc.sync.dma_start(out=gf[:], in_=gfin[:]), writes=['gf'], dma=True)
                for i in range(NGRP * 4):
                    r = i % 2
                    P.op('sp', lambda i=i, r=r: nc.sync.dma_start(out=xf[:, r, :], in_=XA[i * 128:(i + 1) * 128, :]), reads=['XA'], writes=[('xf', r)], dma=True)
                    P.op('act', lambda r=r: nc.scalar.activation(out=jf[:], in_=xf[:, r, :], func=AF.Square, accum_out=sf[:, r, 0:1]), reads=[('xf', r)], writes=['jf', ('sf', r)])
                    P.op('act', lambda r=r: nc.scalar.activation(out=sf[:, r, 1:2], in_=sf[:, r, 0:1], func=AF.Sqrt, scale=1.0 / D, bias=cvec[:, 3:4]), reads=[('sf', r)], writes=[('sf', r)])
                    P.op('dve', lambda r=r: nc.vector.reciprocal(out=sf[:, r, 2:3], in_=sf[:, r, 1:2]), reads=[('sf', r)], writes=[('sf', r)])
                    P.op('dve', lambda r=r: nc.vector.scalar_tensor_tensor(out=xf[:, r, :], in0=xf[:, r, :], scalar=sf[:, r, 2:3], in1=gf[:], op0=ALU.mult, op1=ALU.mult), reads=[('xf', r), ('sf', r), 'gf'], writes=[('xf', r)])
                    P.op('sp', lambda i=i, r=r: nc.sync.dma_start(out=out[i * 128:(i + 1) * 128, :], in_=xf[:, r, :]), reads=[('xf', r)], writes=['out'], dma=True)
                P.flush()
        P.flush()
    return nc


BF = ml_dtypes.bfloat16

def _consts():
    c = {}
    c["identf"] = np.eye(128, dtype=np.float32); c["identb"] = np.eye(128).astype(BF)
    sw = np.zeros((128, 128), np.float32)
    for m in range(128): sw[(m + 64) % 128, m] = 1
    c["sw128"] = sw.astype(BF)
    sw = np.zeros((128, 128), np.float32)
    for m in range(128): sw[(m // 64) * 64 + ((m % 64) + 32) % 64, m] = 1
    c["sw64"] = sw.astype(BF)
    t = np.arange(128)
    tri = (t[None, :] <= t[:, None]).astype(np.float32)
    c["tri01"] = tri; c["atri01"] = 1 - tri
    tabs = []
    for (W, d) in PAT:
        for delta in range(-3, W // 128 + 4):
            sk = np.arange(128)[:, None]; tq = np.arange(128)[None, :]
            diff = 128 * delta + tq - sk
            ok = (diff >= 0) & (diff <= W) & (diff % d == 0)
            tabs.append(np.where(ok, 0.0, NEG))
    c["dtab"] = np.stack(tabs, 1).astype(BF)
    cv = np.zeros((128, 8), np.float32); j = np.arange(128)
    cv[:, 0] = 1.0 / (10000.0 ** ((2 * (j % 64)).astype(np.float32) / 128))
    cv[:, 1] = 1.0 / (10000.0 ** ((2 * (j % 32)).astype(np.float32) / 64))
    cv[:, 2] = np.where(j < 64, -1.0, 1.0); cv[:, 3] = 1e-5; cv[:, 4] = np.where((j % 64) < 32, -1.0, 1.0)
    c["cvec"] = cv
    c["pow2"] = np.broadcast_to(0.5 ** np.arange(24, dtype=np.float32), (128, 24)).copy()
    return c


def kernel(x, c, positions, w_mod, b_mod, g_norm1, g_norm2, w_in, g_kv, w_kv_up, w_proj_a, w_proj_b, w_out,
           w_router, b_router, w_moe1, b_moe1, w_moe2, b_moe2, g_final):
    NL = 2; NE = 32
    f = lambda a: np.ascontiguousarray(np.asarray(a))
    x = f(x); c = f(c); positions = f(positions); b_mod = f(b_mod); b_moe1 = f(b_moe1)
    shared = dict(_consts())
    shared["w_mod"] = f(w_mod); shared["w_in"] = f(w_in); shared["w_kv_up"] = f(w_kv_up)
    shared["w_proj_a"] = f(w_proj_a); shared["w_proj_b"] = f(w_proj_b); shared["w_out"] = f(w_out)
    shared["w_router"] = f(w_router); shared["w_moe1"] = f(w_moe1); shared["w_moe2"] = f(w_moe2); shared["b_moe2"] = f(b_moe2)
    shared["brow"] = f(np.broadcast_to(np.asarray(b_router)[:, None, :], (NL, 128, NE)))
    b1g = b_moe1[:, :, 0::2].reshape(NL, NE, 16, 128).transpose(0, 1, 3, 2)
    b1l = b_moe1[:, :, 1::2].reshape(NL, NE, 16, 128).transpose(0, 1, 3, 2)
    shared["b1T"] = f(np.concatenate([b1g, b1l], -1))
    shared["gfin"] = f(np.broadcast_to(np.asarray(g_final)[None, :], (128, D)))
    shared["bmodT"] = f(b_mod.reshape(NL, 96, 128).transpose(0, 2, 1))
    gt = np.stack([b_mod[:, 2 * D:3 * D], b_mod[:, 5 * D:6 * D]], 1)
    shared["bmodgt"] = f(np.broadcast_to(gt[:, :, None, :], (NL, 2, 128, D)))
    shared["g1T"] = f(np.asarray(g_norm1).reshape(NL, 16, 128).transpose(0, 2, 1))
    shared["g2T"] = f(np.asarray(g_norm2).reshape(NL, 16, 128).transpose(0, 2, 1))
    shared["gkvT"] = f(np.asarray(g_kv).reshape(NL, 4, 128).transpose(0, 2, 1))
    in_maps = []
    for b in range(4):
        m = dict(shared)
        m["x"] = f(x[b]); m["cT"] = f(c[b].reshape(16, 128).T); m["pos"] = f(positions[b][None, :].astype(np.int32))
        in_maps.append(m)
    nc = build_program(NL=NL, NE=NE, phases="0ABCF")
    res = run_bass_kernel_spmd(nc, in_maps, core_ids=list(range(4)))
    return np.stack([np.asarray(r["out"]) for r in res.results], 0).astype(np.float32)
```
